# Optimizing a Trainium2 kernel written in Bass

```python
import math
import jax, jax.numpy as jnp
from jax import lax
import numpy as np

D_MODEL = 1024
BATCH = 8
SEQ = 4096
DEPTH = 1

N_ATT_HEADS = 4
ATT_DK = 64
ATT_DV = 2 * ATT_DK
ATT_QK_W = N_ATT_HEADS * 2 * ATT_DK
ATT_V_W = N_ATT_HEADS * ATT_DV
Q_BLOCK = 128
CHUNK = 128
SGU_GROUPS = 4
SGU_GROUP_DIM = 128
SGU_W = SGU_GROUPS * SGU_GROUP_DIM
IN_W = 2 * ATT_QK_W + ATT_V_W + 2 * SGU_W
BRANCH_W = ATT_V_W + SGU_W
N_EXPERTS = 16
CAP_FACTOR = 2
D_FF_EXPERT = 2048
EPS = 1e-6

kernel_name = "hybrid_diffattn_sgu_ec_moe_encoder"


def rms_norm(x, g):
    xf = x.astype(jnp.float32)
    y = xf * lax.rsqrt(jnp.mean(xf * xf, axis=-1, keepdims=True) + EPS)
    return (y * g.astype(jnp.float32)).astype(x.dtype)


def layer_norm(x, g, b):
    xf = x.astype(jnp.float32)
    mu = jnp.mean(xf, axis=-1, keepdims=True)
    xc = xf - mu
    y = xc * lax.rsqrt(jnp.mean(xc * xc, axis=-1, keepdims=True) + EPS)
    return (y * g.astype(jnp.float32) + b.astype(jnp.float32)).astype(x.dtype)


def alibi_slopes(n_heads):
    return jnp.array([2.0 ** (-8.0 * (i + 1) / n_heads) for i in range(n_heads)], dtype=jnp.float32)


def diff_attention(q, k, v, lam, slopes):
    B, S = q.shape[0], q.shape[1]
    nb = S // Q_BLOCK
    q = q * (ATT_DK ** -0.5)
    qb = q.reshape(B, nb, Q_BLOCK, N_ATT_HEADS, 2, ATT_DK).transpose(1, 0, 3, 4, 2, 5)
    kt = k.transpose(0, 2, 3, 1, 4)
    vt = v.transpose(0, 2, 1, 3)
    kpos = jnp.arange(S, dtype=jnp.float32)

    def block(args):
        qblk, start = args
        qpos = start + jnp.arange(Q_BLOCK, dtype=jnp.float32)
        bias = -slopes[:, None, None, None] * jnp.abs(qpos[:, None] - kpos[None, :])
        s = jnp.einsum('bhcqd,bhckd->bhcqk', qblk, kt).astype(jnp.float32) + bias
        p = jax.nn.softmax(s, axis=-1)
        a = p[:, :, 0] - lam * p[:, :, 1]
        return jnp.einsum('bhqk,bhkd->bhqd', a.astype(vt.dtype), vt)

    starts = jnp.arange(nb, dtype=jnp.float32) * Q_BLOCK
    o = lax.map(block, (qb, starts))
    return o.transpose(1, 0, 3, 2, 4).reshape(B, S, N_ATT_HEADS, ATT_DV)


def spatial_gating(u, s, ln_g, ln_b, w_s, b_s):
    B, S, _ = s.shape
    s = layer_norm(s, ln_g, ln_b)
    sc = s.reshape(B, S // CHUNK, CHUNK, SGU_GROUPS, SGU_GROUP_DIM)
    z = jnp.einsum('gts,bcsgd->bctgd', w_s, sc) + b_s.T[None, None, :, :, None]
    return u * z.reshape(B, S, SGU_W)


def expert_choice_ffn(h, w_router, w_gate, w_up, w_down):
    B, S, D = h.shape
    cap = CAP_FACTOR * S // N_EXPERTS
    logits = jnp.einsum('bsd,de->bse', h, w_router).astype(jnp.float32)
    aff = jax.nn.softmax(logits, axis=-1)
    g, idx = lax.top_k(aff.transpose(0, 2, 1), cap)
    xs = jax.vmap(lambda hb, ib: hb[ib])(h, idx)
    a = jnp.einsum('becd,edf->becf', xs, w_gate)
    u = jnp.einsum('becd,edf->becf', xs, w_up)
    y = jnp.einsum('becf,efd->becd', jax.nn.silu(a) * u, w_down)
    y = y * g[..., None].astype(y.dtype)
    return jax.vmap(lambda yb, ib: jnp.zeros((S, D), yb.dtype).at[ib.reshape(-1)].add(yb.reshape(-1, D)))(y, idx)


def setup_inputs(seed: int = 0) -> dict:
    key = jax.random.key(seed)
    ks = jax.random.split(key, 24)
    f32 = jnp.float32
    L, D = DEPTH, D_MODEL
    nrm = lambda k, shape, scale: jax.random.normal(k, shape, f32) * scale
    return {
        "x": jax.random.normal(ks[0], (BATCH, SEQ, D), f32),
        "norm_mix_g": 1.0 + nrm(ks[1], (L, D), 0.02),
        "w_in": nrm(ks[2], (L, D, IN_W), D ** -0.5),
        "w_gate": nrm(ks[3], (L, D, 2 * D), D ** -0.5),
        "b_gate": nrm(ks[4], (L, 2 * D), 0.02),
        "lam_q1": nrm(ks[5], (L, ATT_DK), 0.1),
        "lam_k1": nrm(ks[6], (L, ATT_DK), 0.1),
        "lam_q2": nrm(ks[7], (L, ATT_DK), 0.1),
        "lam_k2": nrm(ks[8], (L, ATT_DK), 0.1),
        "subln_g": 1.0 + nrm(ks[9], (L, ATT_DV), 0.02),
        "sgu_ln_g": 1.0 + nrm(ks[10], (L, SGU_W), 0.02),
        "sgu_ln_b": nrm(ks[11], (L, SGU_W), 0.02),
        "sgu_w": nrm(ks[12], (L, SGU_GROUPS, CHUNK, CHUNK), CHUNK ** -0.5),
        "sgu_b": 1.0 + nrm(ks[13], (L, SGU_GROUPS, CHUNK), 0.02),
        "w_branch": nrm(ks[14], (L, BRANCH_W, D), (BRANCH_W // 2) ** -0.5),
        "w_out": nrm(ks[15], (L, D, D), D ** -0.5),
        "norm_ffn_g": 1.0 + nrm(ks[16], (L, D), 0.02),
        "w_router": nrm(ks[17], (L, D, N_EXPERTS), D ** -0.5),
        "w_e_gate": nrm(ks[18], (L, N_EXPERTS, D, D_FF_EXPERT), D ** -0.5),
        "w_e_up": nrm(ks[19], (L, N_EXPERTS, D, D_FF_EXPERT), D ** -0.5),
        "w_e_down": nrm(ks[20], (L, N_EXPERTS, D_FF_EXPERT, D), D_FF_EXPERT ** -0.5),
        "final_norm_g": 1.0 + nrm(ks[21], (D,), 0.02),
    }


def reference(x, norm_mix_g, w_in, w_gate, b_gate, lam_q1, lam_k1, lam_q2, lam_k2, subln_g,
              sgu_ln_g, sgu_ln_b, sgu_w, sgu_b, w_branch, w_out, norm_ffn_g, w_router,
              w_e_gate, w_e_up, w_e_down, final_norm_g):
    B, S, D = x.shape
    slopes = alibi_slopes(N_ATT_HEADS)
    for l in range(DEPTH):
        h = rms_norm(x, norm_mix_g[l])
        proj = jnp.einsum('bsd,dn->bsn', h, w_in[l])
        o1 = ATT_QK_W
        o2 = o1 + ATT_QK_W
        o3 = o2 + ATT_V_W
        o4 = o3 + SGU_W
        q = proj[..., :o1].reshape(B, S, N_ATT_HEADS, 2, ATT_DK)
        k = proj[..., o1:o2].reshape(B, S, N_ATT_HEADS, 2, ATT_DK)
        v = proj[..., o2:o3].reshape(B, S, N_ATT_HEADS, ATT_DV)
        g_sgu = jax.nn.gelu(proj[..., o3:], approximate=False)
        u, s = g_sgu[..., :SGU_W], g_sgu[..., SGU_W:]

        lambda_init = 0.8 - 0.6 * math.exp(-0.3 * l)
        lam = (jnp.exp(jnp.sum(lam_q1[l].astype(jnp.float32) * lam_k1[l].astype(jnp.float32)))
               - jnp.exp(jnp.sum(lam_q2[l].astype(jnp.float32) * lam_k2[l].astype(jnp.float32)))
               + lambda_init)
        att = diff_attention(q, k, v, lam, slopes)
        att = (rms_norm(att, subln_g[l]) * (1.0 - lambda_init)).reshape(B, S, ATT_V_W)

        sgu = spatial_gating(u, s, sgu_ln_g[l], sgu_ln_b[l], sgu_w[l], sgu_b[l])

        gates = jax.nn.sigmoid(jnp.einsum('bsd,dn->bsn', h, w_gate[l]) + b_gate[l])
        y_att = jnp.einsum('bsc,cd->bsd', att, w_branch[l, :ATT_V_W])
        y_sgu = jnp.einsum('bsc,cd->bsd', sgu, w_branch[l, ATT_V_W:])
        mixed = gates[..., :D] * y_att + gates[..., D:] * y_sgu
        x = x + jnp.einsum('bsd,de->bse', mixed, w_out[l])

        h2 = rms_norm(x, norm_ffn_g[l])
        x = x + expert_choice_ffn(h2, w_router[l], w_e_gate[l], w_e_up[l], w_e_down[l])
    return rms_norm(x, final_norm_g)
```

```python
import math
import numpy as np
import ml_dtypes
import concourse.bass as bass
import concourse.mybir as mybir
from concourse.bass_utils import run_bass_kernel_spmd

F32 = mybir.dt.float32
BF16 = mybir.dt.bfloat16
I32 = mybir.dt.int32
U8 = mybir.dt.uint8
AF = mybir.ActivationFunctionType
ALU = mybir.AluOpType
AX = mybir.AxisListType

D = 1024
H = 4
NE = 16
DFF = 2048
EPS = 1e-6
SLOPES = [2.0 ** (-8.0 * (i + 1) / H) for i in range(H)]
LAMBDA_INIT = 0.8 - 0.6 * math.exp(-0.3 * 0)
ARENA = 207 * 1024
SERIAL = True
NBIS = 27


def _dsz(dt):
    return {F32: 4, BF16: 2, I32: 4, U8: 1}[dt]


class Buf:
    __slots__ = ("ap", "w", "r")

    def __init__(self, ap):
        self.ap = ap
        self.w = []
        self.r = []


class Eng:
    def __init__(self, nc, name, h, nd):
        self.name = name
        self.h = h
        self.is_pe = name == "pe"
        self.sem = nc.alloc_semaphore("sem_" + name)
        self.cnt = 0
        self.seen = {}
        self.dsems = [nc.alloc_semaphore(f"dsem_{name}_{i}") for i in range(nd)]
        self.dcnt = [0] * nd
        self.dnext = 0


class FW:
    def __init__(self, nc):
        self.nc = nc
        self.pe = Eng(nc, "pe", nc.tensor, 0)
        self.act = Eng(nc, "act", nc.scalar, 0)
        self.dve = Eng(nc, "dve", nc.vector, 0)
        self.pool = Eng(nc, "pool", nc.gpsimd, 40)
        self.sp = Eng(nc, "sp", nc.sync, 40)
        self.engs = [self.pe, self.act, self.dve, self.pool, self.sp]
        self.by_name = {e.name: e for e in self.engs}
        self.last_tok = None
        self.glast = None

    def _wait(self, eng, tok):
        sem, val, key = tok
        if eng.seen.get(key, 0) >= val:
            return
        if key in self.by_name:
            assert val <= self.by_name[key].cnt, ("wait on not-yet-emitted signal", key, val, self.by_name[key].cnt)
        eng.h.wait_ge(sem, val)
        eng.seen[key] = val

    def _deps(self, eng, reads, writes):
        for b in reads:
            for t in b.w:
                if eng.is_pe and t[2] == "pe":
                    continue
                self._wait(eng, t)
        for b in writes:
            for t in b.w + b.r:
                if t[2] == eng.name:
                    continue
                self._wait(eng, t)

    def _commit(self, tok, reads, writes):
        for b in reads:
            b.r = [t for t in b.r if t[2] != tok[2]] + [tok]
        for b in writes:
            b.w = [tok]
            b.r = []

    def op(self, eng, fn, reads=(), writes=(), signal=True):
        self._deps(eng, reads, writes)
        if SERIAL:
            signal = True
            if self.glast is not None and self.glast[2] != eng.name:
                self._wait(eng, self.glast)
        inst = fn()
        if signal:
            eng.cnt += 1
            inst.then_inc(eng.sem, 1)
            tok = (eng.sem, eng.cnt, eng.name)
        else:
            tok = (eng.sem, eng.cnt + 1, eng.name)
        self._commit(tok, reads, writes)
        self.glast = tok
        return tok

    def dma(self, eng, fn, reads=(), writes=()):
        self._deps(eng, reads, writes)
        i = eng.dnext
        eng.dnext = (eng.dnext + 1) % len(eng.dsems)
        sem = eng.dsems[i]
        key = f"d_{eng.name}_{i}"
        if eng.dcnt[i] > 0:
            self._wait(eng, (sem, eng.dcnt[i], key))
        inst = fn()
        eng.dcnt[i] += 16
        inst.then_inc(sem, 16)
        tok = (sem, eng.dcnt[i], key)
        self._commit(tok, reads, writes)
        self.last_tok = tok
        return tok

    def barrier(self):
        toks = []
        for e in self.engs:
            if e.cnt:
                toks.append((e.sem, e.cnt, e.name))
            for i, s in enumerate(e.dsems):
                if e.dcnt[i]:
                    toks.append((s, e.dcnt[i], f"d_{e.name}_{i}"))
        for e in self.engs:
            for t in toks:
                if t[2] == e.name:
                    continue
                self._wait(e, t)


class Bump:
    def __init__(self, nc, base, start, end):
        self.nc = nc
        self.base = base
        self.pos = start
        self.end = end
        self.n = 0

    def alloc(self, name, shape, dt):
        nbytes = int(np.prod(shape[1:])) * _dsz(dt)
        nbytes = (nbytes + 31) // 32 * 32
        off = self.pos
        self.pos += nbytes
        assert self.pos <= self.end, (name, self.pos, self.end)
        Bump_counter[0] += 1
        return self.nc.alloc_sbuf_tensor_at(f"{name}_{Bump_counter[0]}", list(shape), dt, offset=self.base + off).ap()


Bump_counter = [0]


def build(S, dbg=False, stop_after=None):
    NT = S // 128
    NCH = S // 512
    CAP = 2 * S // NE
    NCT = CAP // 128
    assert S % 512 == 0 and CAP % 128 == 0 and CAP <= 512

    nc = bass.Bass("TRN2", target_bir_lowering=False)
    fw = FW(nc)
    pe, act, dve, pool, sp = fw.pe, fw.act, fw.dve, fw.pool, fw.sp
    V = nc.vector
    A = nc.scalar
    T = nc.tensor
    G = nc.gpsimd
    SY = nc.sync

    def finish():
        for e_ in fw.engs:
            if e_.cnt and e_ is not sp:
                sp.h.wait_ge(e_.sem, e_.cnt)
        for e_ in (sp, pool):
            for i, sm_ in enumerate(e_.dsems):
                if e_.dcnt[i]:
                    sp.h.wait_ge(sm_, e_.dcnt[i])
        return nc, dbg_out

    def din(name, shape, dt=F32):
        return nc.dram_tensor(name, list(shape), dt, kind="ExternalInput").ap()

    x_d = din("x", [S, D])
    gmix_d = din("norm_mix_g", [1, D])
    w_in_d = din("w_in", [D, 2560])
    w_gate_d = din("w_gate", [D, 2 * D])
    bgT_d = din("bgT", [128, 16])
    lam_d = [din(n, [1, 64]) for n in ("lam_q1", "lam_k1", "lam_q2", "lam_k2")]
    subg_d = din("subln_gT", [128, 1])
    lng_d = din("sgu_ln_g", [1, 512])
    lnb_d = din("sgu_ln_b", [1, 512])
    wsT_d = din("sgu_wT", [128, 4, 128])
    bs_d = din("sgu_b", [1, 512])
    wbr_d = din("w_branch", [D, D])
    wout_d = din("w_out", [D, D])
    gffn_d = din("norm_ffn_g", [1, D])
    wr_d = din("w_router", [D, NE])
    weg_d = din("w_e_gate", [NE, D, DFF])
    weu_d = din("w_e_up", [NE, D, DFF])
    wed_d = din("w_e_down", [NE, DFF, D])
    gfin_d = din("final_norm_g", [1, D])
    qaug_d = din("c_qaug", [32, S], BF16)
    kaugp_d = din("c_kaugp", [32, S], BF16)
    kaugm_d = din("c_kaugm", [32, S], BF16)
    identb_d = din("c_identb", [128, 128], BF16)
    identf_d = din("c_identf", [128, 128])
    cdiag_d = din("c_cdiag", [128, 128], BF16)
    utri_d = din("c_utri", [128, 128], BF16)
    iota_d = din("c_iota", [128, 512])
    metac_d = din("c_meta", [128, NE, NT, 5], BF16)

    out_d = nc.dram_tensor("out", [S, D], F32, kind="ExternalOutput").ap()
    acc_d = nc.dram_tensor("acc_d", [S, D], F32).ap()
    h2_d = nc.dram_tensor("h2_d", [S, D], BF16).ap()
    dbg_out = {}

    def dout(name, shape, dt=F32):
        ap = nc.dram_tensor("dbg_" + name, list(shape), dt, kind="ExternalOutput").ap()
        dbg_out[name] = ap
        return ap

    nc.alloc_sbuf_tensor("arena", [128, ARENA], U8)
    base = nc.lookup_mloc("arena").addr
    KB = 1024

    banks = [Buf(nc.alloc_psum_tensor(f"bank{i}", [128, 512], F32).ap()) for i in range(8)]

    CK = 16 * KB
    cst = Bump(nc, base, 0, CK)
    identb = Buf(cst.alloc("identb", [128, 128], BF16))
    identf = Buf(cst.alloc("identf", [128, 128], F32))
    onesb = Buf(cst.alloc("onesb", [128, 128], BF16))
    cdiag = Buf(cst.alloc("cdiag", [128, 128], BF16))
    utri = Buf(cst.alloc("utri", [128, 128], BF16))
    mhalf = Buf(cst.alloc("mhalf", [128, 8], F32))
    gbc = Buf(cst.alloc("gbc", [128, D], F32))
    neglam = Buf(cst.alloc("neglam", [128, 1], F32))
    gsub = Buf(cst.alloc("gsub", [128, 1], F32))
    small = Buf(cst.alloc("small", [128, 8], F32))
    epsb = Buf(cst.alloc("epsb", [128, 1], F32))
    lamt = Buf(cst.alloc("lamt", [128, 4, 64], F32))
    logits_ap = cst.alloc("logits", [128, NE, NT], F32)
    logits = Buf(logits_ap)
    aff_ap = cst.alloc("aff", [128, NE, NT], F32)
    aff = Buf(aff_ap)

    fw.dma(sp, lambda: SY.dma_start(out=identb.ap, in_=identb_d), writes=[identb])
    fw.dma(sp, lambda: SY.dma_start(out=identf.ap, in_=identf_d), writes=[identf])
    fw.dma(sp, lambda: SY.dma_start(out=cdiag.ap, in_=cdiag_d), writes=[cdiag])
    fw.dma(sp, lambda: SY.dma_start(out=utri.ap, in_=utri_d), writes=[utri])
    fw.dma(sp, lambda: SY.dma_start(out=gbc.ap, in_=gmix_d.to_broadcast([128, D])), writes=[gbc])
    for i in range(4):
        fw.dma(sp, lambda i=i: SY.dma_start(out=lamt.ap[:, i, :], in_=lam_d[i].to_broadcast([128, 64])), writes=[lamt])
    fw.dma(sp, lambda: SY.dma_start(out=gsub.ap, in_=subg_d), writes=[gsub])
    fw.op(dve, lambda: V.memset(onesb.ap, 1.0), writes=[onesb])
    fw.op(dve, lambda: V.memset(epsb.ap, EPS), writes=[epsb])
    pass
    fw.op(dve, lambda: V.memset(mhalf.ap, -0.5), writes=[mhalf])
    for i_ in range(2):
        fw.op(dve, lambda i_=i_: V.tensor_tensor(out=lamt.ap[:, 2 * i_, :], in0=lamt.ap[:, 2 * i_, :], in1=lamt.ap[:, 2 * i_ + 1, :], op=ALU.mult), reads=[lamt], writes=[lamt])
        fw.op(dve, lambda i_=i_: V.tensor_reduce(out=small.ap[:, i_:i_ + 1], in_=lamt.ap[:, 2 * i_, :], axis=AX.X, op=ALU.add), reads=[lamt], writes=[small])
    fw.op(act, lambda: A.activation(out=small.ap[:, 2:4], in_=small.ap[:, 0:2], func=AF.Exp), reads=[small], writes=[small])
    fw.op(dve, lambda: V.tensor_tensor(out=small.ap[:, 4:5], in0=small.ap[:, 3:4], in1=small.ap[:, 2:3], op=ALU.subtract), reads=[small], writes=[small])
    fw.op(dve, lambda: V.tensor_scalar(out=neglam.ap, in0=small.ap[:, 4:5], scalar1=-LAMBDA_INIT, scalar2=None, op0=ALU.add), reads=[small], writes=[neglam])
    fw.op(dve, lambda: V.tensor_scalar(out=gsub.ap, in0=gsub.ap, scalar1=1.0 - LAMBDA_INIT, scalar2=None, op0=ALU.mult), reads=[gsub], writes=[gsub])

    def rms_tile(xin, ssb, msb, rsb, outb, out_dtype_bf16_buf=None):
        junk = rms_junk
        fw.op(act, lambda: A.activation(out=junk.ap, in_=xin.ap, func=AF.Square, accum_out=ssb.ap), reads=[xin], writes=[junk, ssb])
        fw.op(dve, lambda: V.tensor_scalar(out=msb.ap, in0=ssb.ap, scalar1=1.0 / D, scalar2=EPS, op0=ALU.mult, op1=ALU.add), reads=[ssb], writes=[msb])
        fw.op(pool, lambda: G.tensor_tensor(out=rsb.ap, in0=msb.ap, in1=mhalf.ap[:, 0:1], op=ALU.pow), reads=[msb, mhalf], writes=[rsb])
        fw.op(dve, lambda: V.scalar_tensor_tensor(out=outb.ap, in0=xin.ap, scalar=rsb.ap, in1=gbc.ap, op0=ALU.mult, op1=ALU.mult),
              reads=[xin, rsb, gbc], writes=[outb])

    def transpose_bf_tile(src, bank, dst_ap, dstbuf, evac_eng):
        pv = bank.ap.bitcast(BF16)
        for k in range(8):
            fw.op(pe, lambda k=k: T.transpose(out=pv[:, k * 128:(k + 1) * 128], in_=src.ap[:, k * 128:(k + 1) * 128], identity=identb.ap),
                  reads=[src, identb], writes=[bank], signal=(k == 7))
        src_v = pv.rearrange("p (k t) -> p k t", k=8)
        if evac_eng is act:
            fw.op(act, lambda: A.copy(out=dst_ap, in_=src_v), reads=[bank], writes=[dstbuf])
        else:
            fw.op(dve, lambda: V.tensor_copy(out=dst_ap, in_=src_v), reads=[bank], writes=[dstbuf])

    attT_ap = nc.alloc_sbuf_tensor_at("attT", [128, 4, S], BF16, offset=base + CK).ap()
    hT_off = CK + S * 8
    hT_ap = nc.alloc_sbuf_tensor_at("hT", [128, 8, S], BF16, offset=base + hT_off).ap()
    attT = [[Buf(attT_ap[:, h, c * 512:(c + 1) * 512]) for c in range(NCH)] for h in range(H)]
    hT = [Buf(hT_ap[:, :, t * 128:(t + 1) * 128]) for t in range(NT)]

    def hT_chunk_bufs(c):
        return hT[4 * c:4 * c + 4]

    ab = Bump(nc, base, hT_off + S * 16, ARENA)
    rms_junk = Buf(ab.alloc("junk", [128, D], BF16))
    qk_ap = {}
    qk = {}
    for nm in ("Q0", "Q1", "K0p", "K0m", "K1p", "K1m"):
        ap_ = ab.alloc(nm, [96, S], BF16)
        qk_ap[nm] = ap_
        qk[nm] = [Buf(ap_[0:64, c * 512:(c + 1) * 512]) for c in range(NCH)]
    aug = {nm: Buf(qk_ap[nm][64:96, :]) for nm in qk_ap}
    Vh_ap = ab.alloc("Vh", [128, NT, 128], BF16)
    Vh = [Buf(Vh_ap[:, 4 * c:4 * c + 4, :]) for c in range(NCH)]
    ptile = [[Buf(ab.alloc(f"pt{i}{j}", [128, 512], BF16)) for j in range(2)] for i in range(2)]
    wqkv = [Buf(ab.alloc(f"wqkv{i}", [128, 8, 384], BF16)) for i in range(1)] * 2
    xin = [Buf(ab.alloc(f"xin{i}", [128, D], F32)) for i in range(2)]
    xn = [Buf(ab.alloc(f"xn{i}", [128, D], BF16)) for i in range(2)]
    ss_ap = ab.alloc("ss", [128, NT], F32)
    ms_ap = ab.alloc("ms", [128, NT], F32)
    rs_ap = ab.alloc("rs", [128, NT], F32)
    cmb = [Buf(ab.alloc(f"cmb{i}", [128, 512], F32)) for i in range(5)]
    sqb = Buf(ab.alloc("sqb", [128, 512], BF16))

    for nm, src in (("Q0", qaug_d), ("Q1", qaug_d), ("K0p", kaugp_d), ("K1p", kaugp_d), ("K0m", kaugm_d), ("K1m", kaugm_d)):
        fw.dma(sp, lambda nm=nm, src=src: SY.dma_start(out=qk_ap[nm][64:96, :], in_=src), writes=[aug[nm]])

    def load_wqkv(h):
        wb = wqkv[h % 2]
        for j, off in enumerate((0, 512, 1024)):
            fw.dma(pool, lambda j=j, off=off: G.dma_start(out=wb.ap[:, :, j * 128:(j + 1) * 128],
                                                          in_=w_in_d[:, off + h * 128: off + (h + 1) * 128].rearrange("(k p) n -> p k n", p=128)),
                   writes=[wb])

    if stop_after == "A0":
        return finish()
    load_wqkv(0)
    if stop_after == "A1":
        return finish()
    for t in range(NT):
        if stop_after == "A2" and t == 1:
            return finish()
        xi = xin[t % 2]
        fw.dma(sp, lambda t=t, xi=xi: SY.dma_start(out=xi.ap, in_=x_d[t * 128:(t + 1) * 128, :]), writes=[xi])
        ssb, msb, rsb = Buf(ss_ap[:, t:t + 1]), Buf(ms_ap[:, t:t + 1]), Buf(rs_ap[:, t:t + 1])
        rms_tile(xi, ssb, msb, rsb, xn[t % 2])
        transpose_bf_tile(xn[t % 2], banks[t % 2], hT[t].ap, hT[t], act)

    if dbg:
        o = dout("hT", [128, 8, S], BF16)
        fw.dma(sp, lambda: SY.dma_start(out=o, in_=hT_ap), reads=hT)

    if stop_after == "A":
        return finish()
    sb_banks = [[banks[0], banks[1]], [banks[2], banks[3]]]
    accO = [banks[4], banks[6]]
    accL = [banks[5], banks[7]]
    for h in range(H):
        wb = wqkv[h % 2]
        qscale = 1.0 / (8.0 * SLOPES[h])
        pbank = 0
        for c in range(NCH):
            hcb = hT_chunk_bufs(c)
            bq = banks[pbank % 4]; pbank += 1
            for k in range(8):
                fw.op(pe, lambda k=k, bq=bq: T.matmul(bq.ap, lhsT=wb.ap[:, k, 0:128], rhs=hT_ap[:, k, c * 512:(c + 1) * 512], start=(k == 0), stop=(k == 7)),
                      reads=[wb] + hcb, writes=[bq], signal=(k == 7))
            fw.op(act, lambda bq=bq: A.activation(out=qk["Q0"][c].ap, in_=bq.ap[0:64, :], func=AF.Copy, scale=qscale), reads=[bq], writes=[qk["Q0"][c]])
            fw.op(dve, lambda bq=bq: V.tensor_scalar(out=qk["Q1"][c].ap, in0=bq.ap[64:128, :], scalar1=qscale, scalar2=None, op0=ALU.mult), reads=[bq], writes=[qk["Q1"][c]])
            bk = banks[pbank % 4]; pbank += 1
            for k in range(8):
                fw.op(pe, lambda k=k, bk=bk: T.matmul(bk.ap, lhsT=wb.ap[:, k, 128:256], rhs=hT_ap[:, k, c * 512:(c + 1) * 512], start=(k == 0), stop=(k == 7)),
                      reads=[wb] + hcb, writes=[bk], signal=(k == 7))
            fw.op(act, lambda bk=bk: A.copy(out=qk["K0p"][c].ap, in_=bk.ap[0:64, :]), reads=[bk], writes=[qk["K0p"][c]])
            fw.op(dve, lambda bk=bk: V.tensor_copy(out=qk["K0m"][c].ap, in_=bk.ap[0:64, :]), reads=[bk], writes=[qk["K0m"][c]])
            fw.op(act, lambda bk=bk: A.copy(out=qk["K1p"][c].ap, in_=bk.ap[64:128, :]), reads=[bk], writes=[qk["K1p"][c]])
            fw.op(dve, lambda bk=bk: V.tensor_copy(out=qk["K1m"][c].ap, in_=bk.ap[64:128, :]), reads=[bk], writes=[qk["K1m"][c]])
            bv = banks[pbank % 4]; pbank += 1
            for i in range(4):
                t = 4 * c + i
                for k in range(8):
                    fw.op(pe, lambda k=k, i=i, t=t, bv=bv: T.matmul(bv.ap[:, i * 128:(i + 1) * 128], lhsT=hT_ap[:, k, t * 128:(t + 1) * 128], rhs=wb.ap[:, k, 256:384],
                                                                     start=(k == 0), stop=(k == 7)), reads=[wb, hT[t]], writes=[bv], signal=(k == 7 and i == 3))
            fw.op(dve, lambda bv=bv: V.tensor_copy(out=Vh[c].ap, in_=bv.ap.rearrange("p (a b) -> p a b", a=4)), reads=[bv], writes=[Vh[c]])

        if stop_after == "B0":
            return finish()
        if h + 1 < H:
            load_wqkv(h + 1)
        slope = SLOPES[h]

        def scores(qc, kt, par):
            kc = kt // 4
            ks = slice(kt * 128, (kt + 1) * 128)
            for comp in range(2):
                bnk = sb_banks[par][comp]
                Qn = "Q%d" % comp
                if kc != qc:
                    Kn = ("K%dp" if qc > kc else "K%dm") % comp
                    fw.op(pe, lambda bnk=bnk, Kn=Kn, Qn=Qn: T.matmul(bnk.ap, lhsT=qk_ap[Kn][0:96, ks], rhs=qk_ap[Qn][0:96, qc * 512:(qc + 1) * 512], start=True, stop=True),
                          reads=[qk[Kn][kc], qk[Qn][qc], aug[Kn], aug[Qn]], writes=[bnk])
                else:
                    kb = kt % 4
                    order = [i for i in range(4) if i != kb] + [kb]
                    for i in order:
                        Kn = ("K%dp" if i >= kb else "K%dm") % comp
                        qs = slice(qc * 512 + i * 128, qc * 512 + (i + 1) * 128)
                        fw.op(pe, lambda bnk=bnk, Kn=Kn, Qn=Qn, qs=qs, i=i: T.matmul(bnk.ap[:, i * 128:(i + 1) * 128], lhsT=qk_ap[Kn][0:96, ks], rhs=qk_ap[Qn][0:96, qs],
                                                                                      start=True, stop=(i != kb)),
                              reads=[qk[Kn][kc], qk[Qn][qc], aug[Kn], aug[Qn]], writes=[bnk], signal=False)
                        if i == kb:
                            fw.op(pe, lambda bnk=bnk, i=i: T.matmul(bnk.ap[:, i * 128:(i + 1) * 128], lhsT=identb.ap, rhs=cdiag.ap, start=False, stop=True),
                                  reads=[identb, cdiag], writes=[bnk])

        for qc in range(NCH):
            scores(qc, 0, 0)
            for kt in range(NT):
                par = kt % 2
                if kt + 1 < NT:
                    scores(qc, kt + 1, 1 - par)
                for comp in range(2):
                    bnk = sb_banks[par][comp]
                    pt = ptile[par][comp]
                    fw.op(act, lambda bnk=bnk, pt=pt: A.activation(out=pt.ap, in_=bnk.ap, func=AF.Exp, scale=slope), reads=[bnk], writes=[pt])
                for comp in range(2):
                    pt = ptile[par][comp]
                    fw.op(pe, lambda pt=pt, comp=comp: T.matmul(accO[comp].ap, lhsT=Vh_ap[:, kt, :], rhs=pt.ap, start=(kt == 0), stop=(kt == NT - 1)),
                          reads=[pt, Vh[kt // 4]], writes=[accO[comp]], signal=(kt == NT - 1))
                    fw.op(pe, lambda pt=pt, comp=comp: T.matmul(accL[comp].ap, lhsT=onesb.ap, rhs=pt.ap, start=(kt == 0), stop=(kt == NT - 1)),
                          reads=[pt, onesb], writes=[accL[comp]], signal=(kt == NT - 1))
            if stop_after == "B1":
                return finish()
            r1, t1, r2, t2, aa = cmb
            fw.op(dve, lambda: V.reciprocal(out=r1.ap, in_=accL[0].ap), reads=[accL[0]], writes=[r1])
            fw.op(dve, lambda: V.tensor_tensor(out=t1.ap, in0=accO[0].ap, in1=r1.ap, op=ALU.mult), reads=[accO[0], r1], writes=[t1])
            fw.op(dve, lambda: V.reciprocal(out=r2.ap, in_=accL[1].ap), reads=[accL[1]], writes=[r2])
            fw.op(dve, lambda: V.tensor_tensor(out=t2.ap, in0=accO[1].ap, in1=r2.ap, op=ALU.mult), reads=[accO[1], r2], writes=[t2])
            fw.op(dve, lambda: V.scalar_tensor_tensor(out=aa.ap, in0=t2.ap, scalar=neglam.ap, in1=t1.ap, op0=ALU.mult, op1=ALU.add), reads=[t2, t1, neglam], writes=[aa])
            fw.op(dve, lambda: V.tensor_tensor(out=sqb.ap, in0=aa.ap, in1=aa.ap, op=ALU.mult), reads=[aa], writes=[sqb])
            sbk = sb_banks[0][0]
            fw.op(pe, lambda: T.matmul(sbk.ap, lhsT=onesb.ap, rhs=sqb.ap, start=True, stop=True), reads=[onesb, sqb], writes=[sbk])
            fw.op(act, lambda: A.activation(out=r1.ap, in_=sbk.ap, func=AF.Ln, scale=1.0 / 128, bias=epsb.ap), reads=[sbk, epsb], writes=[r1])
            fw.op(act, lambda: A.activation(out=r2.ap, in_=r1.ap, func=AF.Exp, scale=-0.5), reads=[r1], writes=[r2])
            fw.op(dve, lambda: V.scalar_tensor_tensor(out=attT[h][qc].ap, in0=aa.ap, scalar=gsub.ap, in1=r2.ap, op0=ALU.mult, op1=ALU.mult),
                  reads=[aa, gsub, r2], writes=[attT[h][qc]])
            if stop_after == "B2":
                return finish()

    if dbg:
        o = dout("attT", [128, 4, S], BF16)
        fw.dma(sp, lambda: SY.dma_start(out=o, in_=attT_ap), reads=[b for hb in attT for b in hb])

    if stop_after == "B":
        return finish()
    fw.barrier()
    sgu_off = hT_off + S * 16
    sguT_ap = nc.alloc_sbuf_tensor_at("sguT", [128, 4, S], BF16, offset=base + sgu_off).ap()
    sguT = [Buf(sguT_ap[:, :, t * 128:(t + 1) * 128]) for t in range(NT)]
    cb = Bump(nc, base, sgu_off + S * 8, ARENA)
    wu = Buf(cb.alloc("wu", [128, 8, 512], BF16))
    wsw = Buf(cb.alloc("ws", [128, 8, 512], BF16))
    wsT = Buf(cb.alloc("wsT", [128, 4, 128], BF16))
    lng = Buf(cb.alloc("lng", [128, 512], F32))
    lnb = Buf(cb.alloc("lnb", [128, 512], F32))
    bsb = Buf(cb.alloc("bsb", [128, 512], F32))
    uT = [Buf(cb.alloc(f"uT{i}", [128, 4, 512], BF16)) for i in range(2)]
    gs = [Buf(cb.alloc(f"gs{i}", [128, 512], F32)) for i in range(2)]
    sc1 = [Buf(cb.alloc(f"sc1{i}", [128, 512], F32)) for i in range(2)]
    scb = [Buf(cb.alloc(f"scb{i}", [128, 512], BF16)) for i in range(2)]
    zb = [Buf(cb.alloc(f"zb{i}", [128, 512], F32)) for i in range(2)]
    st_ap = cb.alloc("bnst", [128, NT, 6], F32)
    mv_ap = cb.alloc("bnmv", [128, NT, 2], F32)
    lms_ap = cb.alloc("lms", [128, NT], F32)
    lrs_ap = cb.alloc("lrs", [128, NT], F32)

    fw.dma(pool, lambda: G.dma_start(out=wu.ap, in_=w_in_d[:, 1536:2048].rearrange("(k p) n -> p k n", p=128)), writes=[wu])
    fw.dma(pool, lambda: G.dma_start(out=wsw.ap, in_=w_in_d[:, 2048:2560].rearrange("(k p) n -> p k n", p=128)), writes=[wsw])
    fw.dma(pool, lambda: G.dma_start(out=wsT.ap, in_=wsT_d), writes=[wsT])
    fw.dma(sp, lambda: SY.dma_start(out=lng.ap, in_=lng_d.to_broadcast([128, 512])), writes=[lng])
    fw.dma(sp, lambda: SY.dma_start(out=lnb.ap, in_=lnb_d.to_broadcast([128, 512])), writes=[lnb])
    fw.dma(sp, lambda: SY.dma_start(out=bsb.ap, in_=bs_d.to_broadcast([128, 512])), writes=[bsb])

    pass
    for c in range(NCH):
        hcb = hT_chunk_bufs(c)
        u = uT[c % 2]
        for cc in range(4):
            bu = banks[cc]
            for k in range(8):
                fw.op(pe, lambda k=k, cc=cc, bu=bu: T.matmul(bu.ap, lhsT=wu.ap[:, k, cc * 128:(cc + 1) * 128], rhs=hT_ap[:, k, c * 512:(c + 1) * 512], start=(k == 0), stop=(k == 7)),
                      reads=[wu] + hcb, writes=[bu], signal=(k == 7))
            fw.op(act, lambda cc=cc, bu=bu: A.activation(out=u.ap[:, cc, :], in_=bu.ap, func=AF.Gelu), reads=[bu], writes=[u])
        for i in range(4):
            t = 4 * c + i
            bs_ = banks[4 + (t % 2)]
            bz = banks[6 + (t % 2)]
            g_, s1_, sb_, z_ = gs[t % 2], sc1[t % 2], scb[t % 2], zb[t % 2]
            for k in range(8):
                fw.op(pe, lambda k=k, t=t, bs_=bs_: T.matmul(bs_.ap, lhsT=hT_ap[:, k, t * 128:(t + 1) * 128], rhs=wsw.ap[:, k, :], start=(k == 0), stop=(k == 7)),
                      reads=[wsw, hT[t]], writes=[bs_], signal=(k == 7))
            fw.op(act, lambda: A.activation(out=g_.ap, in_=bs_.ap, func=AF.Gelu), reads=[bs_], writes=[g_])
            stb, mvb = Buf(st_ap[:, t, :]), Buf(mv_ap[:, t, :])
            lmsb, lrsb = Buf(lms_ap[:, t:t + 1]), Buf(lrs_ap[:, t:t + 1])
            fw.op(dve, lambda: V.bn_stats(out=stb.ap, in_=g_.ap), reads=[g_], writes=[stb])
            fw.op(dve, lambda: V.bn_aggr(out=mvb.ap, in_=stb.ap), reads=[stb], writes=[mvb])
            fw.op(dve, lambda: V.tensor_scalar(out=lmsb.ap, in0=mvb.ap[:, 1:2], scalar1=EPS, scalar2=None, op0=ALU.add), reads=[mvb], writes=[lmsb])
            fw.op(pool, lambda: G.tensor_tensor(out=lrsb.ap, in0=lmsb.ap, in1=mhalf.ap[:, 0:1], op=ALU.pow), reads=[lmsb, mhalf], writes=[lrsb])
            fw.op(dve, lambda: V.tensor_scalar(out=s1_.ap, in0=g_.ap, scalar1=mvb.ap[:, 0:1], scalar2=lrsb.ap, op0=ALU.subtract, op1=ALU.mult),
                  reads=[g_, mvb, lrsb], writes=[s1_])
            fw.op(dve, lambda: V.tensor_tensor(out=s1_.ap, in0=s1_.ap, in1=lng.ap, op=ALU.mult), reads=[s1_, lng], writes=[s1_])
            fw.op(dve, lambda: V.tensor_tensor(out=sb_.ap, in0=s1_.ap, in1=lnb.ap, op=ALU.add), reads=[s1_, lnb], writes=[sb_])
            for g in range(4):
                fw.op(pe, lambda g=g: T.matmul(bz.ap[:, g * 128:(g + 1) * 128], lhsT=sb_.ap[:, g * 128:(g + 1) * 128], rhs=wsT.ap[:, g, :], start=True, stop=True),
                      reads=[sb_, wsT], writes=[bz], signal=(g == 3))
            fw.op(dve, lambda: V.tensor_tensor(out=z_.ap, in0=bz.ap, in1=bsb.ap, op=ALU.add), reads=[bz, bsb], writes=[z_])
            fw.op(dve, lambda i=i: V.tensor_tensor(out=sguT[t].ap, in0=z_.ap.rearrange("p (g t) -> p g t", g=4), in1=u.ap[:, :, i * 128:(i + 1) * 128], op=ALU.mult),
                  reads=[z_, u], writes=[sguT[t]])

    if dbg:
        o = dout("sguT", [128, 4, S], BF16)
        fw.dma(sp, lambda: SY.dma_start(out=o, in_=sguT_ap), reads=sguT)

    if stop_after == "C":
        return finish()
    fw.barrier()
    if S * 16 >= 64 * KB:
        w_off, d_start = hT_off, sgu_off + S * 8
    else:
        w_off = sgu_off + S * 8
        d_start = w_off + 64 * KB
    wg_ap = nc.alloc_sbuf_tensor_at("wg", [128, 8, 2048], BF16, offset=base + w_off).ap()
    wbr_ap = nc.alloc_sbuf_tensor_at("wbr", [128, 8, 1024], BF16, offset=base + w_off + 32 * KB).ap()
    wo_ap = nc.alloc_sbuf_tensor_at("wo", [128, 8, 1024], BF16, offset=base + w_off + 48 * KB).ap()
    db = Bump(nc, base, d_start, ARENA)
    wgb = [Buf(wg_ap[:, :, i * 1024:(i + 1) * 1024]) for i in range(2)]
    wbrb = Buf(wbr_ap)
    wob = Buf(wo_ap)
    for i in range(2):
        fw.dma(pool, lambda i=i: G.dma_start(out=wgb[i].ap, in_=w_gate_d[:, i * 1024:(i + 1) * 1024].rearrange("(k p) n -> p k n", p=128)), writes=[wgb[i]])
    fw.dma(pool, lambda: G.dma_start(out=wbrb.ap, in_=wbr_d.rearrange("(k p) n -> p k n", p=128)), writes=[wbrb])
    fw.dma(pool, lambda: G.dma_start(out=wob.ap, in_=wout_d.rearrange("(k p) n -> p k n", p=128)), writes=[wob])
    bgt = Buf(db.alloc("bgt", [128, 16], F32))
    wr = Buf(db.alloc("wr", [128, 8, NE], F32))
    gffn = Buf(db.alloc("gffn", [128, D], F32))
    dxin = [Buf(db.alloc(f"dxin{i}", [128, D], F32)) for i in range(4)]
    dxn = [Buf(db.alloc(f"dxn{i}", [128, D], BF16)) for i in range(1)] * 2
    hTc_ap = db.alloc("hTc", [128, 8, 512], BF16)
    hTc = [Buf(hTc_ap[:, :, i * 128:(i + 1) * 128]) for i in range(4)]
    mixT_ap = db.alloc("mixT", [128, 8, 512], BF16)
    mixT = [Buf(mixT_ap[:, j, :]) for j in range(8)]
    gAB = [Buf(db.alloc(f"gAB{i}", [128, 512], F32)) for i in range(2)] * 2
    x1 = [Buf(db.alloc(f"x1{i}", [128, D], F32)) for i in range(1)] * 2
    h2f = Buf(db.alloc("h2f", [128, D], F32))
    h2b = [Buf(db.alloc(f"h2b{i}", [128, D], BF16)) for i in range(1)] * 2
    h2T = Buf(db.alloc("h2T", [128, 8, 128], F32))
    dss_ap = db.alloc("dss", [128, 3, 2 * NT], F32)
    rms_junk = Buf(db.alloc("junkd", [128, D], BF16))
    fw.dma(sp, lambda: SY.dma_start(out=bgt.ap, in_=bgT_d), writes=[bgt])
    fw.dma(sp, lambda: SY.dma_start(out=wr.ap, in_=wr_d.rearrange("(k p) e -> p k e", p=128)), writes=[wr])
    fw.dma(sp, lambda: SY.dma_start(out=gffn.ap, in_=gffn_d.to_broadcast([128, D])), writes=[gffn])
    acc_rows = [Buf(acc_d[t * 128:(t + 1) * 128, :]) for t in range(NT)]
    h2_all = Buf(h2_d)

    pass
    for c in range(NCH):
        for i in range(4):
            t = 4 * c + i
            fw.dma(sp, lambda t=t, i=i: SY.dma_start(out=dxin[i].ap, in_=x_d[t * 128:(t + 1) * 128, :]), writes=[dxin[i]])
            ssb, msb, rsb = Buf(dss_ap[:, 0, t:t + 1]), Buf(dss_ap[:, 1, t:t + 1]), Buf(dss_ap[:, 2, t:t + 1])
            rms_tile(dxin[i], ssb, msb, rsb, dxn[t % 2])
            transpose_bf_tile(dxn[t % 2], banks[t % 2], hTc[i].ap, hTc[i], act)
        for j in range(8):
            bA = banks[(j % 2) * 4 + 0]
            bB = banks[(j % 2) * 4 + 1]
            bYA = banks[(j % 2) * 4 + 2]
            bYB = banks[(j % 2) * 4 + 3]
            js = slice(j * 128, (j + 1) * 128)
            for k in range(8):
                fw.op(pe, lambda k=k, bA=bA: T.matmul(bA.ap, lhsT=wg_ap[:, k, j * 128:(j + 1) * 128], rhs=hTc_ap[:, k, :], start=(k == 0), stop=(k == 7)),
                      reads=[wgb[0]] + hTc, writes=[bA], signal=(k == 7))
            for k in range(8):
                fw.op(pe, lambda k=k, bB=bB: T.matmul(bB.ap, lhsT=wg_ap[:, k, 1024 + j * 128:1024 + (j + 1) * 128], rhs=hTc_ap[:, k, :], start=(k == 0), stop=(k == 7)),
                      reads=[wgb[1]] + hTc, writes=[bB], signal=(k == 7))
            for k in range(4):
                fw.op(pe, lambda k=k, bYA=bYA: T.matmul(bYA.ap, lhsT=wbr_ap[:, k, js], rhs=attT_ap[:, k, c * 512:(c + 1) * 512], start=(k == 0), stop=(k == 3)),
                      reads=[wbrb, attT[k][c]], writes=[bYA], signal=(k == 3))
            for k in range(4):
                fw.op(pe, lambda k=k, bYB=bYB: T.matmul(bYB.ap, lhsT=wbr_ap[:, 4 + k, js], rhs=sguT_ap[:, k, c * 512:(c + 1) * 512], start=(k == 0), stop=(k == 3)),
                      reads=[wbrb] + sguT[4 * c:4 * c + 4], writes=[bYB], signal=(k == 3))
            gA, gB = gAB[(j % 2) * 2], gAB[(j % 2) * 2 + 1]
            fw.op(act, lambda bA=bA, gA=gA: A.activation(out=gA.ap, in_=bA.ap, func=AF.Sigmoid, bias=bgt.ap[:, j:j + 1]), reads=[bA, bgt], writes=[gA])
            fw.op(act, lambda bB=bB, gB=gB: A.activation(out=gB.ap, in_=bB.ap, func=AF.Sigmoid, bias=bgt.ap[:, 8 + j:9 + j]), reads=[bB, bgt], writes=[gB])
            fw.op(dve, lambda gA=gA, bYA=bYA: V.tensor_tensor(out=gA.ap, in0=gA.ap, in1=bYA.ap, op=ALU.mult), reads=[gA, bYA], writes=[gA])
            fw.op(dve, lambda gB=gB, bYB=bYB: V.tensor_tensor(out=gB.ap, in0=gB.ap, in1=bYB.ap, op=ALU.mult), reads=[gB, bYB], writes=[gB])
            fw.op(dve, lambda gA=gA, gB=gB: V.tensor_tensor(out=mixT[j].ap, in0=gA.ap, in1=gB.ap, op=ALU.add), reads=[gA, gB], writes=[mixT[j]])
        for i in range(4):
            t = 4 * c + i
            xo = x1[t % 2]
            for half in range(2):
                bo = banks[half]
                for j in range(8):
                    fw.op(pe, lambda j=j, bo=bo, half=half: T.matmul(bo.ap, lhsT=mixT_ap[:, j, i * 128:(i + 1) * 128], rhs=wo_ap[:, j, half * 512:(half + 1) * 512],
                                                                      start=(j == 0), stop=(j == 7)), reads=[wob] + mixT, writes=[bo], signal=(j == 7))
                fw.op(dve, lambda bo=bo, half=half: V.tensor_tensor(out=xo.ap[:, half * 512:(half + 1) * 512], in0=bo.ap, in1=dxin[i].ap[:, half * 512:(half + 1) * 512], op=ALU.add),
                      reads=[bo, dxin[i]], writes=[xo])
            fw.dma(sp, lambda t=t, xo=xo: SY.dma_start(out=acc_rows[t].ap, in_=xo.ap), reads=[xo], writes=[acc_rows[t]])
            if c == 0 and i == 0:
                pass
            ssb, msb, rsb = Buf(dss_ap[:, 0, NT + t:NT + t + 1]), Buf(dss_ap[:, 1, NT + t:NT + t + 1]), Buf(dss_ap[:, 2, NT + t:NT + t + 1])
            junk = rms_junk
            fw.op(act, lambda xo=xo: A.activation(out=junk.ap, in_=xo.ap, func=AF.Square, accum_out=ssb.ap), reads=[xo], writes=[junk, ssb])
            fw.op(dve, lambda: V.tensor_scalar(out=msb.ap, in0=ssb.ap, scalar1=1.0 / D, scalar2=EPS, op0=ALU.mult, op1=ALU.add), reads=[ssb], writes=[msb])
            fw.op(pool, lambda: G.tensor_tensor(out=rsb.ap, in0=msb.ap, in1=mhalf.ap[:, 0:1], op=ALU.pow), reads=[msb, mhalf], writes=[rsb])
            fw.op(dve, lambda xo=xo: V.scalar_tensor_tensor(out=h2f.ap, in0=xo.ap, scalar=rsb.ap, in1=gffn.ap, op0=ALU.mult, op1=ALU.mult), reads=[xo, rsb, gffn], writes=[h2f])
            hb = h2b[t % 2]
            fw.op(act, lambda hb=hb: A.copy(out=hb.ap, in_=h2f.ap), reads=[h2f], writes=[hb])
            fw.dma(sp, lambda t=t, hb=hb: SY.dma_start(out=h2_d[t * 128:(t + 1) * 128, :], in_=hb.ap), reads=[hb], writes=[h2_all])
            bt = [banks[2], banks[3]]
            for k in range(8):
                fw.op(pe, lambda k=k: T.transpose(out=bt[k // 4].ap[:, (k % 4) * 128:(k % 4 + 1) * 128], in_=h2f.ap[:, k * 128:(k + 1) * 128], identity=identf.ap),
                      reads=[h2f, identf], writes=[bt[k // 4]], signal=(k % 4 == 3))
            fw.op(act, lambda: A.copy(out=h2T.ap[:, 0:4, :], in_=bt[0].ap.rearrange("p (k t) -> p k t", k=4)), reads=[bt[0]], writes=[h2T])
            fw.op(dve, lambda: V.tensor_copy(out=h2T.ap[:, 4:8, :], in_=bt[1].ap.rearrange("p (k t) -> p k t", k=4)), reads=[bt[1], h2T], writes=[h2T])
            bl = banks[6 + (t % 2)]
            for k in range(8):
                fw.op(pe, lambda k=k, bl=bl: T.matmul(bl.ap[:, 0:NE], lhsT=h2T.ap[:, k, :], rhs=wr.ap[:, k, :], start=(k == 0), stop=(k == 7)), reads=[h2T, wr], writes=[bl], signal=(k == 7))
            fw.op(dve, lambda t=t, bl=bl: V.tensor_copy(out=logits_ap[:, :, t], in_=bl.ap[:, 0:NE]), reads=[bl], writes=[logits])

    if dbg:
        o = dout("logits", [128, NE, NT])
        fw.dma(sp, lambda: SY.dma_start(out=o, in_=logits_ap), reads=[logits])
    fw.barrier()
    if dbg:
        o = dout("x1", [S, D])
        tmpb = dxin[0]
        for t in range(NT):
            fw.dma(sp, lambda t=t: SY.dma_start(out=tmpb.ap, in_=acc_d[t * 128:(t + 1) * 128, :]), reads=[acc_rows[t]], writes=[tmpb])
            fw.dma(sp, lambda t=t: SY.dma_start(out=o[t * 128:(t + 1) * 128, :], in_=tmpb.ap), reads=[tmpb])
        fw.barrier()

    if stop_after == "D":
        return finish()
    mb = Bump(nc, base, CK + 96 * KB, ARENA)
    aff2 = Buf(mb.alloc("aff2", [128, NE, NT], F32))
    mx = Buf(mb.alloc("mx", [128, NT], F32))
    sm = Buf(mb.alloc("sm", [128, NT], F32))
    lo = Buf(mb.alloc("lo", [128, NE], F32))
    mid = Buf(mb.alloc("mid", [128, NE], F32))
    cntb = Buf(mb.alloc("cnt", [128, NE], F32))
    cmpb = Buf(mb.alloc("cmp", [128, NE, NT], BF16))
    maskf = Buf(mb.alloc("maskf", [128, NE, NT], F32))
    rank = Buf(mb.alloc("rank", [128, NE, NT], F32))
    cum = Buf(mb.alloc("cum", [128, NE, NT], F32))
    posm = Buf(mb.alloc("posm", [128, NE, NT], F32))
    gtmp = [Buf(mb.alloc(f"gtmp{i}", [128, NE, NT], F32)) for i in range(2)]
    gpb = Buf(mb.alloc("gpb", [128, NE, NT], BF16))
    onesf = Buf(mb.alloc("onesf", [128, NT], F32))
    meta = Buf(mb.alloc("meta", [128, NE, NT, 5], BF16))
    iota = Buf(mb.alloc("iota", [128, 512], F32))
    moe_small_end = mb.pos

    def bc_t(ap2):
        return ap2.unsqueeze(1).to_broadcast([128, NE, NT])

    def bc_e(ap2):
        return ap2.unsqueeze(2).to_broadcast([128, NE, NT])

    pass
    fw.op(dve, lambda: V.tensor_reduce(out=mx.ap, in_=logits_ap.rearrange("p e t -> p t e"), axis=AX.X, op=ALU.max), reads=[logits], writes=[mx])
    fw.op(dve, lambda: V.tensor_tensor(out=aff_ap, in0=logits_ap, in1=bc_t(mx.ap), op=ALU.subtract), reads=[logits, mx], writes=[aff])
    fw.op(act, lambda: A.activation(out=aff_ap, in_=aff_ap, func=AF.Exp), reads=[aff], writes=[aff])
    fw.op(dve, lambda: V.tensor_reduce(out=sm.ap, in_=aff_ap.rearrange("p e t -> p t e"), axis=AX.X, op=ALU.add), reads=[aff], writes=[sm])
    fw.op(dve, lambda: V.reciprocal(out=sm.ap, in_=sm.ap), reads=[sm], writes=[sm])
    fw.op(dve, lambda: V.tensor_tensor(out=aff2.ap, in0=aff_ap, in1=bc_t(sm.ap), op=ALU.mult), reads=[aff, sm], writes=[aff2])
    fw.dma(sp, lambda: SY.dma_start(out=iota.ap, in_=iota_d), writes=[iota])
    fw.dma(sp, lambda: SY.dma_start(out=meta.ap, in_=metac_d), writes=[meta])
    if dbg:
        o = dout("aff", [128, NE, NT])
        fw.dma(sp, lambda: SY.dma_start(out=o, in_=aff2.ap), reads=[aff2])

    fw.op(dve, lambda: V.memset(lo.ap, 0.0), writes=[lo])
    bq_ = banks[7]
    for it in range(NBIS):
        hstep = 2.0 ** (-(it + 1))
        fw.op(dve, lambda: V.tensor_scalar(out=mid.ap, in0=lo.ap, scalar1=hstep, scalar2=None, op0=ALU.add), reads=[lo], writes=[mid])
        fw.op(dve, lambda: V.tensor_tensor(out=cmpb.ap, in0=aff2.ap, in1=bc_e(mid.ap), op=ALU.is_ge), reads=[aff2, mid], writes=[cmpb])
        fw.op(pe, lambda: T.matmul(bq_.ap[:, 0:NE * NT], lhsT=onesb.ap, rhs=cmpb.ap.rearrange("p e t -> p (e t)"), start=True, stop=True), reads=[onesb, cmpb], writes=[bq_])
        fw.op(dve, lambda: V.tensor_reduce(out=cntb.ap, in_=bq_.ap[:, 0:NE * NT].rearrange("p (e t) -> p e t", e=NE), axis=AX.X, op=ALU.add), reads=[bq_], writes=[cntb])
        fw.op(dve, lambda: V.tensor_scalar(out=cntb.ap, in0=cntb.ap, scalar1=CAP - 0.5, scalar2=hstep, op0=ALU.is_ge, op1=ALU.mult), reads=[cntb], writes=[cntb])
        fw.op(dve, lambda: V.tensor_tensor(out=lo.ap, in0=lo.ap, in1=cntb.ap, op=ALU.add), reads=[lo, cntb], writes=[lo])
    fw.op(dve, lambda: V.tensor_tensor(out=cmpb.ap, in0=aff2.ap, in1=bc_e(lo.ap), op=ALU.is_ge), reads=[aff2, lo], writes=[cmpb])
    fw.op(dve, lambda: V.tensor_tensor(out=maskf.ap, in0=aff2.ap, in1=bc_e(lo.ap), op=ALU.is_ge), reads=[aff2, lo], writes=[maskf])
    b6, b7 = banks[6], banks[7]
    flat = lambda ap3: ap3.rearrange("p e t -> p (e t)")
    fw.op(pe, lambda: T.matmul(b6.ap[:, 0:NE * NT], lhsT=utri.ap, rhs=flat(cmpb.ap), start=True, stop=True), reads=[utri, cmpb], writes=[b6])
    fw.op(pe, lambda: T.matmul(b7.ap[:, 0:NE * NT], lhsT=onesb.ap, rhs=flat(cmpb.ap), start=True, stop=True), reads=[onesb, cmpb], writes=[b7])
    fw.op(dve, lambda: V.memset(onesf.ap, 1.0), writes=[onesf])
    fw.op(dve, lambda: V.tensor_copy(out=flat(rank.ap), in_=b7.ap[:, 0:NE * NT]), reads=[b7], writes=[rank])
    for e in range(NE):
        fw.op(dve, lambda e=e: V.tensor_tensor_scan(out=cum.ap[:, e, :], data0=onesf.ap, data1=rank.ap[:, e, :], initial=0.0, op0=ALU.mult, op1=ALU.add),
              reads=[onesf, rank], writes=[cum])
    fw.op(dve, lambda: V.tensor_tensor(out=cum.ap, in0=cum.ap, in1=rank.ap, op=ALU.subtract), reads=[cum, rank], writes=[cum])
    fw.op(dve, lambda: V.tensor_tensor(out=flat(rank.ap), in0=b6.ap[:, 0:NE * NT], in1=flat(cum.ap), op=ALU.add), reads=[b6, cum], writes=[rank])
    fw.op(dve, lambda: V.tensor_tensor(out=posm.ap, in0=rank.ap, in1=maskf.ap, op=ALU.mult), reads=[rank, maskf], writes=[posm])
    fw.op(dve, lambda: V.tensor_scalar(out=posm.ap, in0=posm.ap, scalar1=-1.0, scalar2=None, op0=ALU.add), reads=[posm], writes=[posm])
    g0, g1 = gtmp
    mv_ = meta.ap
    fw.op(dve, lambda: V.tensor_copy(out=gpb.ap, in_=aff2.ap), reads=[aff2], writes=[gpb])
    fw.op(dve, lambda: V.tensor_copy(out=mv_[:, :, :, 2], in_=gpb.ap), reads=[gpb], writes=[meta])
    fw.op(dve, lambda: V.tensor_tensor(out=g0.ap, in0=aff2.ap, in1=gpb.ap, op=ALU.subtract), reads=[aff2, gpb], writes=[g0])
    fw.op(dve, lambda: V.tensor_copy(out=gpb.ap, in_=g0.ap), reads=[g0], writes=[gpb])
    fw.op(dve, lambda: V.tensor_copy(out=mv_[:, :, :, 3], in_=gpb.ap), reads=[gpb], writes=[meta])
    fw.op(dve, lambda: V.tensor_tensor(out=g1.ap, in0=g0.ap, in1=gpb.ap, op=ALU.subtract), reads=[g0, gpb], writes=[g1])
    fw.op(dve, lambda: V.tensor_copy(out=mv_[:, :, :, 4], in_=g1.ap), reads=[g1], writes=[meta])
    if dbg:
        o = dout("posm", [128, NE, NT])
        fw.dma(sp, lambda: SY.dma_start(out=o, in_=posm.ap), reads=[posm])
        o2 = dout("lo", [128, NE])
        fw.dma(sp, lambda: SY.dma_start(out=o2, in_=lo.ap), reads=[lo])

    if stop_after == "R":
        return finish()
    fw.barrier()
    ring_ap = [nc.alloc_sbuf_tensor_at(f"ring{i}", [128, 8192], BF16, offset=base + CK + i * 16 * KB).ap() for i in range(6)]
    ring = [Buf(a) for a in ring_ap]
    mb2 = Bump(nc, base, moe_small_end, ARENA)
    xs = [Buf(mb2.alloc(f"xs{i}", [128, NCT, D], BF16)) for i in range(2)]
    xsT = [Buf(mb2.alloc(f"xsT{i}", [128, 8, CAP], BF16)) for i in range(2)]
    hTe = Buf(mb2.alloc("hTe", [128, 16, CAP], BF16))
    ysb = [Buf(mb2.alloc(f"ysb{i}", [128, D], F32)) for i in range(2)]
    Pt = [Buf(mb2.alloc(f"Pt{i}", [128, CAP], BF16)) for i in range(3)]
    sil = [Buf(mb2.alloc(f"sil{i}", [128, CAP], F32)) for i in range(2)]
    metaT = Buf(mb2.alloc("metaT", [8, CAP], F32))
    metac = [Buf(mb2.alloc(f"metac{i}", [128, NCT, 5], F32)) for i in range(2)]
    idxf = [Buf(mb2.alloc(f"idxf{i}", [128, NCT], F32)) for i in range(2)]
    idxi = [Buf(mb2.alloc(f"idxi{i}", [128, NCT], I32)) for i in range(2)]
    gc = [Buf(mb2.alloc(f"gc{i}", [128, NCT], F32)) for i in range(2)]

    units = []
    for e in range(NE):
        for fb in range(4):
            units.append(("gu", e, fb))
        for half in range(2):
            units.append(("d", e, half))
    unit_issued = [0]

    def issue_unit():
        u = unit_issued[0]
        if u >= len(units):
            return
        unit_issued[0] += 1
        kind, e, i = units[u]
        slot = ring[u % 6]
        sap = ring_ap[u % 6]
        if kind == "gu":
            fw.dma(pool, lambda: G.dma_start(out=sap[:, 0:4096].rearrange("p (k f) -> p k f", k=8),
                                             in_=weg_d[e, :, i * 512:(i + 1) * 512].rearrange("(k p) f -> p k f", p=128)), writes=[slot])
            fw.dma(pool, lambda: G.dma_start(out=sap[:, 4096:8192].rearrange("p (k f) -> p k f", k=8),
                                             in_=weu_d[e, :, i * 512:(i + 1) * 512].rearrange("(k p) f -> p k f", p=128)), reads=[], writes=[])
            slot.w = slot.w + [fw.last_tok]
        else:
            fw.dma(pool, lambda: G.dma_start(out=sap.rearrange("p (k n) -> p k n", k=16),
                                             in_=wed_d[e, :, i * 512:(i + 1) * 512].rearrange("(k p) n -> p k n", p=128)), writes=[slot])

    def unit_index(e, kind, i):
        return e * 6 + (i if kind == "gu" else 4 + i)

    def prep_steps(e):
        s = e % 2
        steps = []
        bm = banks[7]

        def step_t(t):
            p_ = Pt[t % 3]
            fw.op(dve, lambda: V.tensor_scalar(out=p_.ap, in0=iota.ap[:, 0:CAP], scalar1=posm.ap[:, e, t:t + 1], scalar2=None, op0=ALU.is_equal), reads=[iota, posm], writes=[p_])
            fw.op(pe, lambda: T.matmul(bm.ap[0:5, 0:CAP], lhsT=meta.ap[:, e, t, :], rhs=p_.ap, start=(t == 0), stop=(t == NT - 1)), reads=[meta, p_], writes=[bm])

        for t in range(NT):
            steps.append(lambda t=t: step_t(t))

        def fin():
            fw.op(dve, lambda: V.tensor_copy(out=metaT.ap[0:5, :], in_=bm.ap[0:5, 0:CAP]), reads=[bm], writes=[metaT])
            for j in range(NCT):
                fw.op(pe, lambda j=j: T.transpose(out=bm.ap[:, 8 * j:8 * j + 5], in_=metaT.ap[0:5, j * 128:(j + 1) * 128], identity=identf.ap[0:5, 0:5]),
                      reads=[metaT, identf], writes=[bm])
            mc = metac[s]
            fw.op(dve, lambda: V.tensor_copy(out=mc.ap, in_=bm.ap[:, 0:8 * NCT].rearrange("p (j f) -> p j f", f=8)[:, :, 0:5]), reads=[bm], writes=[mc])
            fw.op(dve, lambda: V.scalar_tensor_tensor(out=idxf[s].ap, in0=mc.ap[:, :, 0], scalar=128.0, in1=mc.ap[:, :, 1], op0=ALU.mult, op1=ALU.add), reads=[mc], writes=[idxf[s]])
            fw.op(dve, lambda: V.tensor_copy(out=idxi[s].ap, in_=idxf[s].ap), reads=[idxf[s]], writes=[idxi[s]])
            fw.op(dve, lambda: V.tensor_tensor(out=gc[s].ap, in0=mc.ap[:, :, 2], in1=mc.ap[:, :, 3], op=ALU.add), reads=[mc], writes=[gc[s]])
            fw.op(dve, lambda: V.tensor_tensor(out=gc[s].ap, in0=gc[s].ap, in1=mc.ap[:, :, 4], op=ALU.add), reads=[gc[s], mc], writes=[gc[s]])
            for j in range(NCT):
                fw.dma(pool, lambda j=j: G.indirect_dma_start(out=xs[s].ap[:, j, :], out_offset=None, in_=h2_d,
                                                              in_offset=bass.IndirectOffsetOnAxis(ap=idxi[s].ap[:, j:j + 1], axis=0)),
                       reads=[idxi[s], h2_all], writes=[xs[s]])

        steps.append(fin)
        return steps

    def xs_transposes(e):
        s = e % 2
        bt_ = banks[6]
        pv = bt_.ap.bitcast(BF16)
        for j in range(NCT):
            for k in range(8):
                fw.op(pe, lambda j=j, k=k: T.transpose(out=pv[:, k * 128:(k + 1) * 128], in_=xs[s].ap[:, j, k * 128:(k + 1) * 128], identity=identb.ap),
                      reads=[xs[s], identb], writes=[bt_], signal=(k == 7))
            fw.op(act, lambda j=j: A.copy(out=xsT[s].ap[:, :, j * 128:(j + 1) * 128], in_=pv.rearrange("p (k t) -> p k t", k=8)), reads=[bt_], writes=[xsT[s]])

    pass
    for _ in range(6):
        issue_unit()
    for st in prep_steps(0):
        st()
    xs_transposes(0)
    if dbg:
        o = dout("idx0", [128, NCT], I32)
        fw.dma(sp, lambda: SY.dma_start(out=o, in_=idxi[0].ap), reads=[idxi[0]])
        o2 = dout("gc0", [128, NCT])
        fw.dma(sp, lambda: SY.dma_start(out=o2, in_=gc[0].ap), reads=[gc[0]])

    if stop_after == "P":
        return finish()
    scat_toks = []
    for e in range(NE):
        s = e % 2
        nxt = prep_steps(e + 1) if e + 1 < NE else []
        per = (len(nxt) + 15) // 16 if nxt else 0
        for fc in range(16):
            fb, fl = fc // 4, fc % 4
            u = unit_index(e, "gu", fb)
            slot, sap = ring[u % 6], ring_ap[u % 6]
            wgv = sap[:, 0:4096].rearrange("p (k f) -> p k f", k=8)
            wuv = sap[:, 4096:8192].rearrange("p (k f) -> p k f", k=8)
            ba, bu = banks[fc % 2], banks[2 + fc % 2]
            for k in range(8):
                fw.op(pe, lambda k=k: T.matmul(ba.ap[:, 0:CAP], lhsT=wgv[:, k, fl * 128:(fl + 1) * 128], rhs=xsT[s].ap[:, k, :], start=(k == 0), stop=(k == 7)),
                      reads=[slot, xsT[s]], writes=[ba], signal=(k == 7))
            for k in range(8):
                fw.op(pe, lambda k=k: T.matmul(bu.ap[:, 0:CAP], lhsT=wuv[:, k, fl * 128:(fl + 1) * 128], rhs=xsT[s].ap[:, k, :], start=(k == 0), stop=(k == 7)),
                      reads=[slot, xsT[s]], writes=[bu], signal=(k == 7))
            sl = sil[fc % 2]
            fw.op(act, lambda: A.activation(out=sl.ap, in_=ba.ap[:, 0:CAP], func=AF.Silu), reads=[ba], writes=[sl])
            fw.op(dve, lambda: V.tensor_tensor(out=hTe.ap[:, fc, :], in0=sl.ap, in1=bu.ap[:, 0:CAP], op=ALU.mult), reads=[sl, bu], writes=[hTe])
            if fl == 3:
                issue_unit()
            for _ in range(per):
                if nxt:
                    nxt.pop(0)()
        while nxt:
            nxt.pop(0)()
        if e > 0 and scat_toks:
            pass
        for j in range(NCT):
            yo = ysb[j % 2]
            for half in range(2):
                u = unit_index(e, "d", half)
                slot, sap = ring[u % 6], ring_ap[u % 6]
                wdv = sap.rearrange("p (k n) -> p k n", k=16)
                by = banks[4 + half]
                for fc in range(16):
                    fw.op(pe, lambda fc=fc: T.matmul(by.ap, lhsT=hTe.ap[:, fc, j * 128:(j + 1) * 128], rhs=wdv[:, fc, :], start=(fc == 0), stop=(fc == 15)),
                          reads=[slot, hTe], writes=[by], signal=(fc == 15))
                if half == 0:
                    fw.op(act, lambda: A.activation(out=yo.ap[:, 0:512], in_=by.ap, func=AF.Copy, scale=gc[s].ap[:, j:j + 1]), reads=[by, gc[s]], writes=[yo])
                else:
                    fw.op(dve, lambda: V.tensor_scalar(out=yo.ap[:, 512:1024], in0=by.ap, scalar1=gc[s].ap[:, j:j + 1], scalar2=None, op0=ALU.mult), reads=[by, gc[s], yo], writes=[yo])
            fw.dma(pool, lambda j=j, yo=yo: G.indirect_dma_start(out=acc_d, out_offset=bass.IndirectOffsetOnAxis(ap=idxi[s].ap[:, j:j + 1], axis=0),
                                                                  in_=yo.ap, in_offset=None, compute_op=ALU.add),
                   reads=[yo, idxi[s]] + acc_rows, writes=[])
            scat_toks.append(fw.last_tok)
        for b in acc_rows:
            b.w = list(scat_toks[-NCT:])
            b.r = []
        issue_unit()
        issue_unit()
        if e + 1 < NE:
            xs_transposes(e + 1)

    fw.barrier()
    eb = Bump(nc, base, CK, ARENA)
    fx = [Buf(eb.alloc(f"fx{i}", [128, D], F32)) for i in range(3)]
    fo = [Buf(eb.alloc(f"fo{i}", [128, D], F32)) for i in range(3)]
    fss_ap = eb.alloc("fss", [128, 3, NT], F32)
    rms_junk = Buf(eb.alloc("junke", [128, D], BF16))
    fw.dma(sp, lambda: SY.dma_start(out=gbc.ap, in_=gfin_d.to_broadcast([128, D])), writes=[gbc])
    outb = Buf(out_d)
    for t in range(NT):
        xi, xo = fx[t % 3], fo[t % 3]
        fw.dma(sp, lambda t=t, xi=xi: SY.dma_start(out=xi.ap, in_=acc_d[t * 128:(t + 1) * 128, :]), reads=[acc_rows[t]], writes=[xi])
        ssb, msb, rsb = Buf(fss_ap[:, 0, t:t + 1]), Buf(fss_ap[:, 1, t:t + 1]), Buf(fss_ap[:, 2, t:t + 1])
        rms_tile(xi, ssb, msb, rsb, xo)
        fw.dma(sp, lambda t=t, xo=xo: SY.dma_start(out=out_d[t * 128:(t + 1) * 128, :], in_=xo.ap), reads=[xo], writes=[])
    return finish()


def host_consts(S):
    NT = S // 128
    bf = ml_dtypes.bfloat16
    pos = np.arange(S)
    hi = (pos // 64) * 64
    lo = pos % 64
    qaug = np.zeros((32, S), np.float32)
    qaug[0] = -hi; qaug[1] = -lo; qaug[2] = 1; qaug[3] = 1
    kaugp = np.zeros((32, S), np.float32)
    kaugp[0] = 1; kaugp[1] = 1; kaugp[2] = hi; kaugp[3] = lo
    kaugm = -kaugp
    kk = np.arange(128)[:, None]; qq = np.arange(128)[None, :]
    cdiag = -2.0 * np.maximum(kk - qq, 0)
    utri = (kk <= qq).astype(np.float32)
    iota = np.broadcast_to(np.arange(512, dtype=np.float32), (128, 512)).copy()
    meta = np.zeros((128, NE, NT, 5), np.float32)
    meta[:, :, :, 0] = np.arange(NT)[None, None, :]
    meta[:, :, :, 1] = np.arange(128)[:, None, None]
    return {
        "c_qaug": qaug.astype(bf), "c_kaugp": kaugp.astype(bf), "c_kaugm": kaugm.astype(bf),
        "c_identb": np.eye(128, dtype=np.float32).astype(bf), "c_identf": np.eye(128, dtype=np.float32),
        "c_cdiag": cdiag.astype(np.float32).astype(bf), "c_utri": utri.astype(bf), "c_iota": iota,
        "c_meta": meta.astype(bf),
    }


def make_in_maps(inputs, S):
    f = lambda a: np.ascontiguousarray(np.asarray(a, dtype=np.float32))
    shared = {
        "norm_mix_g": f(inputs["norm_mix_g"]).reshape(1, D),
        "w_in": f(inputs["w_in"])[0],
        "w_gate": f(inputs["w_gate"])[0],
        "bgT": np.ascontiguousarray(f(inputs["b_gate"])[0].reshape(16, 128).T),
        "lam_q1": f(inputs["lam_q1"]).reshape(1, 64), "lam_k1": f(inputs["lam_k1"]).reshape(1, 64),
        "lam_q2": f(inputs["lam_q2"]).reshape(1, 64), "lam_k2": f(inputs["lam_k2"]).reshape(1, 64),
        "subln_gT": np.ascontiguousarray(f(inputs["subln_g"]).reshape(1, 128).T),
        "sgu_ln_g": f(inputs["sgu_ln_g"]).reshape(1, 512), "sgu_ln_b": f(inputs["sgu_ln_b"]).reshape(1, 512),
        "sgu_wT": np.ascontiguousarray(np.transpose(f(inputs["sgu_w"])[0], (2, 0, 1))),
        "sgu_b": f(inputs["sgu_b"]).reshape(1, 512),
        "w_branch": f(inputs["w_branch"])[0], "w_out": f(inputs["w_out"])[0],
        "norm_ffn_g": f(inputs["norm_ffn_g"]).reshape(1, D),
        "w_router": f(inputs["w_router"])[0],
        "w_e_gate": f(inputs["w_e_gate"])[0], "w_e_up": f(inputs["w_e_up"])[0], "w_e_down": f(inputs["w_e_down"])[0],
        "final_norm_g": f(inputs["final_norm_g"]).reshape(1, D),
    }
    shared.update(host_consts(S))
    x = f(inputs["x"])
    return [dict(shared, x=x[b]) for b in range(x.shape[0])]


_CACHE = {}


def kernel(**inputs):
    x = np.asarray(inputs["x"])
    B, S, _ = x.shape
    if S not in _CACHE:
        _CACHE[S] = build(S)[0]
    nc = _CACHE[S]
    in_maps = make_in_maps(inputs, S)
    res = run_bass_kernel_spmd(nc, in_maps, core_ids=list(range(B)))
    return np.stack([np.asarray(r["out"], dtype=np.float32) for r in res.results], axis=0)
```

```python
import math
import numpy as np
import ml_dtypes
import concourse.bass as bass
import concourse.mybir as mybir
from concourse.bass_utils import run_bass_kernel_spmd

F32 = mybir.dt.float32
BF16 = mybir.dt.bfloat16
I32 = mybir.dt.int32
U8 = mybir.dt.uint8
AF = mybir.ActivationFunctionType
ALU = mybir.AluOpType
AX = mybir.AxisListType

D = 1024
H = 4
NE = 16
DFF = 2048
EPS = 1e-6
SLOPES = [2.0 ** (-8.0 * (i + 1) / H) for i in range(H)]
LAMBDA_INIT = 0.8 - 0.6 * math.exp(-0.3 * 0)
ARENA = 207 * 1024
SERIAL = True
NBIS = 27


def _dsz(dt):
    return {F32: 4, BF16: 2, I32: 4, U8: 1}[dt]


class Buf:
    __slots__ = ("ap", "w", "r")

    def __init__(self, ap):
        self.ap = ap
        self.w = []
        self.r = []


class Eng:
    def __init__(self, nc, name, h, nd):
        self.name = name
        self.h = h
        self.is_pe = name == "pe"
        self.sem = nc.alloc_semaphore("sem_" + name)
        self.cnt = 0
        self.seen = {}
        self.dsems = [nc.alloc_semaphore(f"dsem_{name}_{i}") for i in range(nd)]
        self.dcnt = [0] * nd
        self.dnext = 0


class FW:
    def __init__(self, nc):
        self.nc = nc
        self.pe = Eng(nc, "pe", nc.tensor, 0)
        self.act = Eng(nc, "act", nc.scalar, 0)
        self.dve = Eng(nc, "dve", nc.vector, 0)
        self.pool = Eng(nc, "pool", nc.gpsimd, 40)
        self.sp = Eng(nc, "sp", nc.sync, 40)
        self.engs = [self.pe, self.act, self.dve, self.pool, self.sp]
        self.by_name = {e.name: e for e in self.engs}
        self.last_tok = None
        self.glast = None

    def _wait(self, eng, tok):
        sem, val, key = tok
        if eng.seen.get(key, 0) >= val:
            return
        if key in self.by_name:
            assert val <= self.by_name[key].cnt, ("wait on not-yet-emitted signal", key, val, self.by_name[key].cnt)
        eng.h.wait_ge(sem, val)
        eng.seen[key] = val

    def _deps(self, eng, reads, writes):
        for b in reads:
            for t in b.w:
                if eng.is_pe and t[2] == "pe":
                    continue
                self._wait(eng, t)
        for b in writes:
            for t in b.w + b.r:
                if t[2] == eng.name:
                    continue
                self._wait(eng, t)

    def _commit(self, tok, reads, writes):
        for b in reads:
            b.r = [t for t in b.r if t[2] != tok[2]] + [tok]
        for b in writes:
            b.w = [tok]
            b.r = []

    def op(self, eng, fn, reads=(), writes=(), signal=True):
        self._deps(eng, reads, writes)
        if SERIAL and not eng.is_pe:
            if self.glast is not None and self.glast[2] != eng.name:
                self._wait(eng, self.glast)
        inst = fn()
        if signal:
            eng.cnt += 1
            inst.then_inc(eng.sem, 1)
            tok = (eng.sem, eng.cnt, eng.name)
        else:
            tok = (eng.sem, eng.cnt + 1, eng.name)
        self._commit(tok, reads, writes)
        if not eng.is_pe:
            self.glast = tok
        return tok

    def dma(self, eng, fn, reads=(), writes=()):
        self._deps(eng, reads, writes)
        i = eng.dnext
        eng.dnext = (eng.dnext + 1) % len(eng.dsems)
        sem = eng.dsems[i]
        key = f"d_{eng.name}_{i}"
        if eng.dcnt[i] > 0:
            self._wait(eng, (sem, eng.dcnt[i], key))
        inst = fn()
        eng.dcnt[i] += 16
        inst.then_inc(sem, 16)
        tok = (sem, eng.dcnt[i], key)
        self._commit(tok, reads, writes)
        self.last_tok = tok
        return tok

    def barrier(self):
        toks = []
        for e in self.engs:
            if e.cnt:
                toks.append((e.sem, e.cnt, e.name))
            for i, s in enumerate(e.dsems):
                if e.dcnt[i]:
                    toks.append((s, e.dcnt[i], f"d_{e.name}_{i}"))
        for e in self.engs:
            for t in toks:
                if t[2] == e.name:
                    continue
                self._wait(e, t)


class Bump:
    def __init__(self, nc, base, start, end):
        self.nc = nc
        self.base = base
        self.pos = start
        self.end = end
        self.n = 0

    def alloc(self, name, shape, dt):
        nbytes = int(np.prod(shape[1:])) * _dsz(dt)
        nbytes = (nbytes + 31) // 32 * 32
        off = self.pos
        self.pos += nbytes
        assert self.pos <= self.end, (name, self.pos, self.end)
        Bump_counter[0] += 1
        return self.nc.alloc_sbuf_tensor_at(f"{name}_{Bump_counter[0]}", list(shape), dt, offset=self.base + off).ap()


Bump_counter = [0]


def build(S, dbg=False, stop_after=None):
    NT = S // 128
    NCH = S // 512
    CAP = 2 * S // NE
    NCT = CAP // 128
    assert S % 512 == 0 and CAP % 128 == 0 and CAP <= 512

    nc = bass.Bass("TRN2", target_bir_lowering=False)
    fw = FW(nc)
    pe, act, dve, pool, sp = fw.pe, fw.act, fw.dve, fw.pool, fw.sp
    V = nc.vector
    A = nc.scalar
    T = nc.tensor
    G = nc.gpsimd
    SY = nc.sync

    def finish():
        for e_ in fw.engs:
            if e_.cnt and e_ is not sp:
                sp.h.wait_ge(e_.sem, e_.cnt)
        for e_ in (sp, pool):
            for i, sm_ in enumerate(e_.dsems):
                if e_.dcnt[i]:
                    sp.h.wait_ge(sm_, e_.dcnt[i])
        return nc, dbg_out

    def din(name, shape, dt=F32):
        return nc.dram_tensor(name, list(shape), dt, kind="ExternalInput").ap()

    x_d = din("x", [S, D])
    gmix_d = din("norm_mix_g", [1, D])
    w_in_d = din("w_in", [D, 2560])
    w_gate_d = din("w_gate", [D, 2 * D])
    bgT_d = din("bgT", [128, 16])
    lam_d = [din(n, [1, 64]) for n in ("lam_q1", "lam_k1", "lam_q2", "lam_k2")]
    subg_d = din("subln_gT", [128, 1])
    lng_d = din("sgu_ln_g", [1, 512])
    lnb_d = din("sgu_ln_b", [1, 512])
    wsT_d = din("sgu_wT", [128, 4, 128])
    bs_d = din("sgu_b", [1, 512])
    wbr_d = din("w_branch", [D, D])
    wout_d = din("w_out", [D, D])
    gffn_d = din("norm_ffn_g", [1, D])
    wr_d = din("w_router", [D, NE])
    weg_d = din("w_e_gate", [NE, D, DFF])
    weu_d = din("w_e_up", [NE, D, DFF])
    wed_d = din("w_e_down", [NE, DFF, D])
    gfin_d = din("final_norm_g", [1, D])
    qaug_d = din("c_qaug", [32, S], BF16)
    kaugp_d = din("c_kaugp", [32, S], BF16)
    kaugm_d = din("c_kaugm", [32, S], BF16)
    identb_d = din("c_identb", [128, 128], BF16)
    identf_d = din("c_identf", [128, 128])
    cdiag_d = din("c_cdiag", [128, 128], BF16)
    utri_d = din("c_utri", [128, 128], BF16)
    iota_d = din("c_iota", [128, 512])
    metac_d = din("c_meta", [128, NE, NT, 5], BF16)

    out_d = nc.dram_tensor("out", [S, D], F32, kind="ExternalOutput").ap()
    acc_d = nc.dram_tensor("acc_d", [S, D], F32).ap()
    h2_d = nc.dram_tensor("h2_d", [S, D], BF16).ap()
    dbg_out = {}

    def dout(name, shape, dt=F32):
        ap = nc.dram_tensor("dbg_" + name, list(shape), dt, kind="ExternalOutput").ap()
        dbg_out[name] = ap
        return ap

    nc.alloc_sbuf_tensor("arena", [128, ARENA], U8)
    base = nc.lookup_mloc("arena").addr
    KB = 1024

    banks = [Buf(nc.alloc_psum_tensor(f"bank{i}", [128, 512], F32).ap()) for i in range(8)]

    CK = 16 * KB
    cst = Bump(nc, base, 0, CK)
    identb = Buf(cst.alloc("identb", [128, 128], BF16))
    identf = Buf(cst.alloc("identf", [128, 128], F32))
    onesb = Buf(cst.alloc("onesb", [128, 128], BF16))
    cdiag = Buf(cst.alloc("cdiag", [128, 128], BF16))
    utri = Buf(cst.alloc("utri", [128, 128], BF16))
    mhalf = Buf(cst.alloc("mhalf", [128, 8], F32))
    gbc = Buf(cst.alloc("gbc", [128, D], F32))
    neglam = Buf(cst.alloc("neglam", [128, 1], F32))
    gsub = Buf(cst.alloc("gsub", [128, 1], F32))
    small = Buf(cst.alloc("small", [128, 8], F32))
    epsb = Buf(cst.alloc("epsb", [128, 1], F32))
    lamt = Buf(cst.alloc("lamt", [128, 4, 64], F32))
    logits_ap = cst.alloc("logits", [128, NE, NT], F32)
    logits = Buf(logits_ap)
    aff_ap = cst.alloc("aff", [128, NE, NT], F32)
    aff = Buf(aff_ap)

    fw.dma(sp, lambda: SY.dma_start(out=identb.ap, in_=identb_d), writes=[identb])
    fw.dma(sp, lambda: SY.dma_start(out=identf.ap, in_=identf_d), writes=[identf])
    fw.dma(sp, lambda: SY.dma_start(out=cdiag.ap, in_=cdiag_d), writes=[cdiag])
    fw.dma(sp, lambda: SY.dma_start(out=utri.ap, in_=utri_d), writes=[utri])
    fw.dma(sp, lambda: SY.dma_start(out=gbc.ap, in_=gmix_d.to_broadcast([128, D])), writes=[gbc])
    for i in range(4):
        fw.dma(sp, lambda i=i: SY.dma_start(out=lamt.ap[:, i, :], in_=lam_d[i].to_broadcast([128, 64])), writes=[lamt])
    fw.dma(sp, lambda: SY.dma_start(out=gsub.ap, in_=subg_d), writes=[gsub])
    fw.op(dve, lambda: V.memset(onesb.ap, 1.0), writes=[onesb])
    fw.op(dve, lambda: V.memset(epsb.ap, EPS), writes=[epsb])
    pass
    fw.op(dve, lambda: V.memset(mhalf.ap, -0.5), writes=[mhalf])
    for i_ in range(2):
        fw.op(dve, lambda i_=i_: V.tensor_tensor(out=lamt.ap[:, 2 * i_, :], in0=lamt.ap[:, 2 * i_, :], in1=lamt.ap[:, 2 * i_ + 1, :], op=ALU.mult), reads=[lamt], writes=[lamt])
        fw.op(dve, lambda i_=i_: V.tensor_reduce(out=small.ap[:, i_:i_ + 1], in_=lamt.ap[:, 2 * i_, :], axis=AX.X, op=ALU.add), reads=[lamt], writes=[small])
    fw.op(act, lambda: A.activation(out=small.ap[:, 2:4], in_=small.ap[:, 0:2], func=AF.Exp), reads=[small], writes=[small])
    fw.op(dve, lambda: V.tensor_tensor(out=small.ap[:, 4:5], in0=small.ap[:, 3:4], in1=small.ap[:, 2:3], op=ALU.subtract), reads=[small], writes=[small])
    fw.op(dve, lambda: V.tensor_scalar(out=neglam.ap, in0=small.ap[:, 4:5], scalar1=-LAMBDA_INIT, scalar2=None, op0=ALU.add), reads=[small], writes=[neglam])
    fw.op(dve, lambda: V.tensor_scalar(out=gsub.ap, in0=gsub.ap, scalar1=1.0 - LAMBDA_INIT, scalar2=None, op0=ALU.mult), reads=[gsub], writes=[gsub])

    def rms_tile(xin, ssb, msb, rsb, outb, out_dtype_bf16_buf=None):
        junk = rms_junk
        fw.op(act, lambda: A.activation(out=junk.ap, in_=xin.ap, func=AF.Square, accum_out=ssb.ap), reads=[xin], writes=[junk, ssb])
        fw.op(dve, lambda: V.tensor_scalar(out=msb.ap, in0=ssb.ap, scalar1=1.0 / D, scalar2=EPS, op0=ALU.mult, op1=ALU.add), reads=[ssb], writes=[msb])
        fw.op(pool, lambda: G.tensor_tensor(out=rsb.ap, in0=msb.ap, in1=mhalf.ap[:, 0:1], op=ALU.pow), reads=[msb, mhalf], writes=[rsb])
        fw.op(dve, lambda: V.scalar_tensor_tensor(out=outb.ap, in0=xin.ap, scalar=rsb.ap, in1=gbc.ap, op0=ALU.mult, op1=ALU.mult),
              reads=[xin, rsb, gbc], writes=[outb])

    def transpose_bf_tile(src, bank, dst_ap, dstbuf, evac_eng):
        pv = bank.ap.bitcast(BF16)
        for k in range(8):
            fw.op(pe, lambda k=k: T.transpose(out=pv[:, k * 128:(k + 1) * 128], in_=src.ap[:, k * 128:(k + 1) * 128], identity=identb.ap),
                  reads=[src, identb], writes=[bank], signal=(k == 7))
        src_v = pv.rearrange("p (k t) -> p k t", k=8)
        if evac_eng is act:
            fw.op(act, lambda: A.copy(out=dst_ap, in_=src_v), reads=[bank], writes=[dstbuf])
        else:
            fw.op(dve, lambda: V.tensor_copy(out=dst_ap, in_=src_v), reads=[bank], writes=[dstbuf])

    attT_ap = nc.alloc_sbuf_tensor_at("attT", [128, 4, S], BF16, offset=base + CK).ap()
    hT_off = CK + S * 8
    hT_ap = nc.alloc_sbuf_tensor_at("hT", [128, 8, S], BF16, offset=base + hT_off).ap()
    attT = [[Buf(attT_ap[:, h, c * 512:(c + 1) * 512]) for c in range(NCH)] for h in range(H)]
    hT = [Buf(hT_ap[:, :, t * 128:(t + 1) * 128]) for t in range(NT)]

    def hT_chunk_bufs(c):
        return hT[4 * c:4 * c + 4]

    ab = Bump(nc, base, hT_off + S * 16, ARENA)
    rms_junk = Buf(ab.alloc("junk", [128, D], BF16))
    qk_ap = {}
    qk = {}
    for nm in ("Q0", "Q1", "K0p", "K0m", "K1p", "K1m"):
        ap_ = ab.alloc(nm, [96, S], BF16)
        qk_ap[nm] = ap_
        qk[nm] = [Buf(ap_[0:64, c * 512:(c + 1) * 512]) for c in range(NCH)]
    aug = {nm: Buf(qk_ap[nm][64:96, :]) for nm in qk_ap}
    Vh_ap = ab.alloc("Vh", [128, NT, 128], BF16)
    Vh = [Buf(Vh_ap[:, 4 * c:4 * c + 4, :]) for c in range(NCH)]
    ptile = [[Buf(ab.alloc(f"pt{i}{j}", [128, 512], BF16)) for j in range(2)] for i in range(2)]
    wqkv = [Buf(ab.alloc(f"wqkv{i}", [128, 8, 384], BF16)) for i in range(1)] * 2
    xin = [Buf(ab.alloc(f"xin{i}", [128, D], F32)) for i in range(2)]
    xn = [Buf(ab.alloc(f"xn{i}", [128, D], BF16)) for i in range(2)]
    ss_ap = ab.alloc("ss", [128, NT], F32)
    ms_ap = ab.alloc("ms", [128, NT], F32)
    rs_ap = ab.alloc("rs", [128, NT], F32)
    cmb = [Buf(ab.alloc(f"cmb{i}", [128, 512], F32)) for i in range(5)]
    sqb = Buf(ab.alloc("sqb", [128, 512], BF16))

    for nm, src in (("Q0", qaug_d), ("Q1", qaug_d), ("K0p", kaugp_d), ("K1p", kaugp_d), ("K0m", kaugm_d), ("K1m", kaugm_d)):
        fw.dma(sp, lambda nm=nm, src=src: SY.dma_start(out=qk_ap[nm][64:96, :], in_=src), writes=[aug[nm]])

    def load_wqkv(h):
        wb = wqkv[h % 2]
        for j, off in enumerate((0, 512, 1024)):
            fw.dma(pool, lambda j=j, off=off: G.dma_start(out=wb.ap[:, :, j * 128:(j + 1) * 128],
                                                          in_=w_in_d[:, off + h * 128: off + (h + 1) * 128].rearrange("(k p) n -> p k n", p=128)),
                   writes=[wb])

    if stop_after == "A0":
        return finish()
    load_wqkv(0)
    if stop_after == "A1":
        return finish()
    for t in range(NT):
        if stop_after == "A2" and t == 1:
            return finish()
        xi = xin[t % 2]
        fw.dma(sp, lambda t=t, xi=xi: SY.dma_start(out=xi.ap, in_=x_d[t * 128:(t + 1) * 128, :]), writes=[xi])
        ssb, msb, rsb = Buf(ss_ap[:, t:t + 1]), Buf(ms_ap[:, t:t + 1]), Buf(rs_ap[:, t:t + 1])
        rms_tile(xi, ssb, msb, rsb, xn[t % 2])
        transpose_bf_tile(xn[t % 2], banks[t % 2], hT[t].ap, hT[t], act)

    if dbg:
        o = dout("hT", [128, 8, S], BF16)
        fw.dma(sp, lambda: SY.dma_start(out=o, in_=hT_ap), reads=hT)

    if stop_after == "A":
        return finish()
    sb_banks = [[banks[0], banks[1]], [banks[2], banks[3]]]
    accO = [banks[4], banks[6]]
    accL = [banks[5], banks[7]]
    for h in range(H):
        wb = wqkv[h % 2]
        qscale = 1.0 / (8.0 * SLOPES[h])
        pbank = 0
        for c in range(NCH):
            hcb = hT_chunk_bufs(c)
            bq = banks[pbank % 4]; pbank += 1
            for k in range(8):
                fw.op(pe, lambda k=k, bq=bq: T.matmul(bq.ap, lhsT=wb.ap[:, k, 0:128], rhs=hT_ap[:, k, c * 512:(c + 1) * 512], start=(k == 0), stop=(k == 7)),
                      reads=[wb] + hcb, writes=[bq], signal=(k == 7))
            fw.op(act, lambda bq=bq: A.activation(out=qk["Q0"][c].ap, in_=bq.ap[0:64, :], func=AF.Copy, scale=qscale), reads=[bq], writes=[qk["Q0"][c]])
            fw.op(dve, lambda bq=bq: V.tensor_scalar(out=qk["Q1"][c].ap, in0=bq.ap[64:128, :], scalar1=qscale, scalar2=None, op0=ALU.mult), reads=[bq], writes=[qk["Q1"][c]])
            bk = banks[pbank % 4]; pbank += 1
            for k in range(8):
                fw.op(pe, lambda k=k, bk=bk: T.matmul(bk.ap, lhsT=wb.ap[:, k, 128:256], rhs=hT_ap[:, k, c * 512:(c + 1) * 512], start=(k == 0), stop=(k == 7)),
                      reads=[wb] + hcb, writes=[bk], signal=(k == 7))
            fw.op(act, lambda bk=bk: A.copy(out=qk["K0p"][c].ap, in_=bk.ap[0:64, :]), reads=[bk], writes=[qk["K0p"][c]])
            fw.op(dve, lambda bk=bk: V.tensor_copy(out=qk["K0m"][c].ap, in_=bk.ap[0:64, :]), reads=[bk], writes=[qk["K0m"][c]])
            fw.op(act, lambda bk=bk: A.copy(out=qk["K1p"][c].ap, in_=bk.ap[64:128, :]), reads=[bk], writes=[qk["K1p"][c]])
            fw.op(dve, lambda bk=bk: V.tensor_copy(out=qk["K1m"][c].ap, in_=bk.ap[64:128, :]), reads=[bk], writes=[qk["K1m"][c]])
            bv = banks[pbank % 4]; pbank += 1
            for i in range(4):
                t = 4 * c + i
                for k in range(8):
                    fw.op(pe, lambda k=k, i=i, t=t, bv=bv: T.matmul(bv.ap[:, i * 128:(i + 1) * 128], lhsT=hT_ap[:, k, t * 128:(t + 1) * 128], rhs=wb.ap[:, k, 256:384],
                                                                     start=(k == 0), stop=(k == 7)), reads=[wb, hT[t]], writes=[bv], signal=(k == 7 and i == 3))
            fw.op(dve, lambda bv=bv: V.tensor_copy(out=Vh[c].ap, in_=bv.ap.rearrange("p (a b) -> p a b", a=4)), reads=[bv], writes=[Vh[c]])

        if stop_after == "B0":
            return finish()
        if h + 1 < H:
            load_wqkv(h + 1)
        slope = SLOPES[h]

        def scores(qc, kt, par):
            kc = kt // 4
            ks = slice(kt * 128, (kt + 1) * 128)
            for comp in range(2):
                bnk = sb_banks[par][comp]
                Qn = "Q%d" % comp
                if kc != qc:
                    Kn = ("K%dp" if qc > kc else "K%dm") % comp
                    fw.op(pe, lambda bnk=bnk, Kn=Kn, Qn=Qn: T.matmul(bnk.ap, lhsT=qk_ap[Kn][0:96, ks], rhs=qk_ap[Qn][0:96, qc * 512:(qc + 1) * 512], start=True, stop=True),
                          reads=[qk[Kn][kc], qk[Qn][qc], aug[Kn], aug[Qn]], writes=[bnk])
                else:
                    kb = kt % 4
                    order = [i for i in range(4) if i != kb] + [kb]
                    for i in order:
                        Kn = ("K%dp" if i >= kb else "K%dm") % comp
                        qs = slice(qc * 512 + i * 128, qc * 512 + (i + 1) * 128)
                        fw.op(pe, lambda bnk=bnk, Kn=Kn, Qn=Qn, qs=qs, i=i: T.matmul(bnk.ap[:, i * 128:(i + 1) * 128], lhsT=qk_ap[Kn][0:96, ks], rhs=qk_ap[Qn][0:96, qs],
                                                                                      start=True, stop=(i != kb)),
                              reads=[qk[Kn][kc], qk[Qn][qc], aug[Kn], aug[Qn]], writes=[bnk], signal=False)
                        if i == kb:
                            fw.op(pe, lambda bnk=bnk, i=i: T.matmul(bnk.ap[:, i * 128:(i + 1) * 128], lhsT=identb.ap, rhs=cdiag.ap, start=False, stop=True),
                                  reads=[identb, cdiag], writes=[bnk])

        for qc in range(NCH):
            scores(qc, 0, 0)
            for kt in range(NT):
                par = kt % 2
                if kt + 1 < NT:
                    scores(qc, kt + 1, 1 - par)
                for comp in range(2):
                    bnk = sb_banks[par][comp]
                    pt = ptile[par][comp]
                    fw.op(act, lambda bnk=bnk, pt=pt: A.activation(out=pt.ap, in_=bnk.ap, func=AF.Exp, scale=slope), reads=[bnk], writes=[pt])
                for comp in range(2):
                    pt = ptile[par][comp]
                    fw.op(pe, lambda pt=pt, comp=comp: T.matmul(accO[comp].ap, lhsT=Vh_ap[:, kt, :], rhs=pt.ap, start=(kt == 0), stop=(kt == NT - 1)),
                          reads=[pt, Vh[kt // 4]], writes=[accO[comp]], signal=(kt == NT - 1))
                    fw.op(pe, lambda pt=pt, comp=comp: T.matmul(accL[comp].ap, lhsT=onesb.ap, rhs=pt.ap, start=(kt == 0), stop=(kt == NT - 1)),
                          reads=[pt, onesb], writes=[accL[comp]], signal=(kt == NT - 1))
            if stop_after == "B1":
                return finish()
            r1, t1, r2, t2, aa = cmb
            fw.op(dve, lambda: V.reciprocal(out=r1.ap, in_=accL[0].ap), reads=[accL[0]], writes=[r1])
            fw.op(dve, lambda: V.tensor_tensor(out=t1.ap, in0=accO[0].ap, in1=r1.ap, op=ALU.mult), reads=[accO[0], r1], writes=[t1])
            fw.op(dve, lambda: V.reciprocal(out=r2.ap, in_=accL[1].ap), reads=[accL[1]], writes=[r2])
            fw.op(dve, lambda: V.tensor_tensor(out=t2.ap, in0=accO[1].ap, in1=r2.ap, op=ALU.mult), reads=[accO[1], r2], writes=[t2])
            fw.op(dve, lambda: V.scalar_tensor_tensor(out=aa.ap, in0=t2.ap, scalar=neglam.ap, in1=t1.ap, op0=ALU.mult, op1=ALU.add), reads=[t2, t1, neglam], writes=[aa])
            fw.op(dve, lambda: V.tensor_tensor(out=sqb.ap, in0=aa.ap, in1=aa.ap, op=ALU.mult), reads=[aa], writes=[sqb])
            sbk = sb_banks[0][0]
            fw.op(pe, lambda: T.matmul(sbk.ap, lhsT=onesb.ap, rhs=sqb.ap, start=True, stop=True), reads=[onesb, sqb], writes=[sbk])
            fw.op(act, lambda: A.activation(out=r1.ap, in_=sbk.ap, func=AF.Ln, scale=1.0 / 128, bias=epsb.ap), reads=[sbk, epsb], writes=[r1])
            fw.op(act, lambda: A.activation(out=r2.ap, in_=r1.ap, func=AF.Exp, scale=-0.5), reads=[r1], writes=[r2])
            fw.op(dve, lambda: V.scalar_tensor_tensor(out=attT[h][qc].ap, in0=aa.ap, scalar=gsub.ap, in1=r2.ap, op0=ALU.mult, op1=ALU.mult),
                  reads=[aa, gsub, r2], writes=[attT[h][qc]])
            if stop_after == "B2":
                return finish()

    if dbg:
        o = dout("attT", [128, 4, S], BF16)
        fw.dma(sp, lambda: SY.dma_start(out=o, in_=attT_ap), reads=[b for hb in attT for b in hb])

    if stop_after == "B":
        return finish()
    fw.barrier()
    sgu_off = hT_off + S * 16
    sguT_ap = nc.alloc_sbuf_tensor_at("sguT", [128, 4, S], BF16, offset=base + sgu_off).ap()
    sguT = [Buf(sguT_ap[:, :, t * 128:(t + 1) * 128]) for t in range(NT)]
    cb = Bump(nc, base, sgu_off + S * 8, ARENA)
    wu = Buf(cb.alloc("wu", [128, 8, 512], BF16))
    wsw = Buf(cb.alloc("ws", [128, 8, 512], BF16))
    wsT = Buf(cb.alloc("wsT", [128, 4, 128], BF16))
    lng = Buf(cb.alloc("lng", [128, 512], F32))
    lnb = Buf(cb.alloc("lnb", [128, 512], F32))
    bsb = Buf(cb.alloc("bsb", [128, 512], F32))
    uT = [Buf(cb.alloc(f"uT{i}", [128, 4, 512], BF16)) for i in range(2)]
    gs = [Buf(cb.alloc(f"gs{i}", [128, 512], F32)) for i in range(2)]
    sc1 = [Buf(cb.alloc(f"sc1{i}", [128, 512], F32)) for i in range(2)]
    scb = [Buf(cb.alloc(f"scb{i}", [128, 512], BF16)) for i in range(2)]
    zb = [Buf(cb.alloc(f"zb{i}", [128, 512], F32)) for i in range(2)]
    st_ap = cb.alloc("bnst", [128, NT, 6], F32)
    mv_ap = cb.alloc("bnmv", [128, NT, 2], F32)
    lms_ap = cb.alloc("lms", [128, NT], F32)
    lrs_ap = cb.alloc("lrs", [128, NT], F32)

    fw.dma(pool, lambda: G.dma_start(out=wu.ap, in_=w_in_d[:, 1536:2048].rearrange("(k p) n -> p k n", p=128)), writes=[wu])
    fw.dma(pool, lambda: G.dma_start(out=wsw.ap, in_=w_in_d[:, 2048:2560].rearrange("(k p) n -> p k n", p=128)), writes=[wsw])
    fw.dma(pool, lambda: G.dma_start(out=wsT.ap, in_=wsT_d), writes=[wsT])
    fw.dma(sp, lambda: SY.dma_start(out=lng.ap, in_=lng_d.to_broadcast([128, 512])), writes=[lng])
    fw.dma(sp, lambda: SY.dma_start(out=lnb.ap, in_=lnb_d.to_broadcast([128, 512])), writes=[lnb])
    fw.dma(sp, lambda: SY.dma_start(out=bsb.ap, in_=bs_d.to_broadcast([128, 512])), writes=[bsb])

    pass
    for c in range(NCH):
        hcb = hT_chunk_bufs(c)
        u = uT[c % 2]
        for cc in range(4):
            bu = banks[cc]
            for k in range(8):
                fw.op(pe, lambda k=k, cc=cc, bu=bu: T.matmul(bu.ap, lhsT=wu.ap[:, k, cc * 128:(cc + 1) * 128], rhs=hT_ap[:, k, c * 512:(c + 1) * 512], start=(k == 0), stop=(k == 7)),
                      reads=[wu] + hcb, writes=[bu], signal=(k == 7))
            fw.op(act, lambda cc=cc, bu=bu: A.activation(out=u.ap[:, cc, :], in_=bu.ap, func=AF.Gelu), reads=[bu], writes=[u])
        for i in range(4):
            t = 4 * c + i
            bs_ = banks[4 + (t % 2)]
            bz = banks[6 + (t % 2)]
            g_, s1_, sb_, z_ = gs[t % 2], sc1[t % 2], scb[t % 2], zb[t % 2]
            for k in range(8):
                fw.op(pe, lambda k=k, t=t, bs_=bs_: T.matmul(bs_.ap, lhsT=hT_ap[:, k, t * 128:(t + 1) * 128], rhs=wsw.ap[:, k, :], start=(k == 0), stop=(k == 7)),
                      reads=[wsw, hT[t]], writes=[bs_], signal=(k == 7))
            fw.op(act, lambda: A.activation(out=g_.ap, in_=bs_.ap, func=AF.Gelu), reads=[bs_], writes=[g_])
            stb, mvb = Buf(st_ap[:, t, :]), Buf(mv_ap[:, t, :])
            lmsb, lrsb = Buf(lms_ap[:, t:t + 1]), Buf(lrs_ap[:, t:t + 1])
            fw.op(dve, lambda: V.bn_stats(out=stb.ap, in_=g_.ap), reads=[g_], writes=[stb])
            fw.op(dve, lambda: V.bn_aggr(out=mvb.ap, in_=stb.ap), reads=[stb], writes=[mvb])
            fw.op(dve, lambda: V.tensor_scalar(out=lmsb.ap, in0=mvb.ap[:, 1:2], scalar1=EPS, scalar2=None, op0=ALU.add), reads=[mvb], writes=[lmsb])
            fw.op(pool, lambda: G.tensor_tensor(out=lrsb.ap, in0=lmsb.ap, in1=mhalf.ap[:, 0:1], op=ALU.pow), reads=[lmsb, mhalf], writes=[lrsb])
            fw.op(dve, lambda: V.tensor_scalar(out=s1_.ap, in0=g_.ap, scalar1=mvb.ap[:, 0:1], scalar2=lrsb.ap, op0=ALU.subtract, op1=ALU.mult),
                  reads=[g_, mvb, lrsb], writes=[s1_])
            fw.op(dve, lambda: V.tensor_tensor(out=s1_.ap, in0=s1_.ap, in1=lng.ap, op=ALU.mult), reads=[s1_, lng], writes=[s1_])
            fw.op(dve, lambda: V.tensor_tensor(out=sb_.ap, in0=s1_.ap, in1=lnb.ap, op=ALU.add), reads=[s1_, lnb], writes=[sb_])
            for g in range(4):
                fw.op(pe, lambda g=g: T.matmul(bz.ap[:, g * 128:(g + 1) * 128], lhsT=sb_.ap[:, g * 128:(g + 1) * 128], rhs=wsT.ap[:, g, :], start=True, stop=True),
                      reads=[sb_, wsT], writes=[bz], signal=(g == 3))
            fw.op(dve, lambda: V.tensor_tensor(out=z_.ap, in0=bz.ap, in1=bsb.ap, op=ALU.add), reads=[bz, bsb], writes=[z_])
            fw.op(dve, lambda i=i: V.tensor_tensor(out=sguT[t].ap, in0=z_.ap.rearrange("p (g t) -> p g t", g=4), in1=u.ap[:, :, i * 128:(i + 1) * 128], op=ALU.mult),
                  reads=[z_, u], writes=[sguT[t]])

    if dbg:
        o = dout("sguT", [128, 4, S], BF16)
        fw.dma(sp, lambda: SY.dma_start(out=o, in_=sguT_ap), reads=sguT)

    if stop_after == "C":
        return finish()
    fw.barrier()
    if S * 16 >= 64 * KB:
        w_off, d_start = hT_off, sgu_off + S * 8
    else:
        w_off = sgu_off + S * 8
        d_start = w_off + 64 * KB
    wg_ap = nc.alloc_sbuf_tensor_at("wg", [128, 8, 2048], BF16, offset=base + w_off).ap()
    wbr_ap = nc.alloc_sbuf_tensor_at("wbr", [128, 8, 1024], BF16, offset=base + w_off + 32 * KB).ap()
    wo_ap = nc.alloc_sbuf_tensor_at("wo", [128, 8, 1024], BF16, offset=base + w_off + 48 * KB).ap()
    db = Bump(nc, base, d_start, ARENA)
    wgb = [Buf(wg_ap[:, :, i * 1024:(i + 1) * 1024]) for i in range(2)]
    wbrb = Buf(wbr_ap)
    wob = Buf(wo_ap)
    for i in range(2):
        fw.dma(pool, lambda i=i: G.dma_start(out=wgb[i].ap, in_=w_gate_d[:, i * 1024:(i + 1) * 1024].rearrange("(k p) n -> p k n", p=128)), writes=[wgb[i]])
    fw.dma(pool, lambda: G.dma_start(out=wbrb.ap, in_=wbr_d.rearrange("(k p) n -> p k n", p=128)), writes=[wbrb])
    fw.dma(pool, lambda: G.dma_start(out=wob.ap, in_=wout_d.rearrange("(k p) n -> p k n", p=128)), writes=[wob])
    bgt = Buf(db.alloc("bgt", [128, 16], F32))
    wr = Buf(db.alloc("wr", [128, 8, NE], F32))
    gffn = Buf(db.alloc("gffn", [128, D], F32))
    dxin = [Buf(db.alloc(f"dxin{i}", [128, D], F32)) for i in range(4)]
    dxn = [Buf(db.alloc(f"dxn{i}", [128, D], BF16)) for i in range(1)] * 2
    hTc_ap = db.alloc("hTc", [128, 8, 512], BF16)
    hTc = [Buf(hTc_ap[:, :, i * 128:(i + 1) * 128]) for i in range(4)]
    mixT_ap = db.alloc("mixT", [128, 8, 512], BF16)
    mixT = [Buf(mixT_ap[:, j, :]) for j in range(8)]
    gAB = [Buf(db.alloc(f"gAB{i}", [128, 512], F32)) for i in range(2)] * 2
    x1 = [Buf(db.alloc(f"x1{i}", [128, D], F32)) for i in range(1)] * 2
    h2f = Buf(db.alloc("h2f", [128, D], F32))
    h2b = [Buf(db.alloc(f"h2b{i}", [128, D], BF16)) for i in range(1)] * 2
    h2T = Buf(db.alloc("h2T", [128, 8, 128], F32))
    dss_ap = db.alloc("dss", [128, 3, 2 * NT], F32)
    rms_junk = Buf(db.alloc("junkd", [128, D], BF16))
    fw.dma(sp, lambda: SY.dma_start(out=bgt.ap, in_=bgT_d), writes=[bgt])
    fw.dma(sp, lambda: SY.dma_start(out=wr.ap, in_=wr_d.rearrange("(k p) e -> p k e", p=128)), writes=[wr])
    fw.dma(sp, lambda: SY.dma_start(out=gffn.ap, in_=gffn_d.to_broadcast([128, D])), writes=[gffn])
    acc_rows = [Buf(acc_d[t * 128:(t + 1) * 128, :]) for t in range(NT)]
    h2_all = Buf(h2_d)

    pass
    for c in range(NCH):
        for i in range(4):
            t = 4 * c + i
            fw.dma(sp, lambda t=t, i=i: SY.dma_start(out=dxin[i].ap, in_=x_d[t * 128:(t + 1) * 128, :]), writes=[dxin[i]])
            ssb, msb, rsb = Buf(dss_ap[:, 0, t:t + 1]), Buf(dss_ap[:, 1, t:t + 1]), Buf(dss_ap[:, 2, t:t + 1])
            rms_tile(dxin[i], ssb, msb, rsb, dxn[t % 2])
            transpose_bf_tile(dxn[t % 2], banks[t % 2], hTc[i].ap, hTc[i], act)
        for j in range(8):
            bA = banks[(j % 2) * 4 + 0]
            bB = banks[(j % 2) * 4 + 1]
            bYA = banks[(j % 2) * 4 + 2]
            bYB = banks[(j % 2) * 4 + 3]
            js = slice(j * 128, (j + 1) * 128)
            for k in range(8):
                fw.op(pe, lambda k=k, bA=bA: T.matmul(bA.ap, lhsT=wg_ap[:, k, j * 128:(j + 1) * 128], rhs=hTc_ap[:, k, :], start=(k == 0), stop=(k == 7)),
                      reads=[wgb[0]] + hTc, writes=[bA], signal=(k == 7))
            for k in range(8):
                fw.op(pe, lambda k=k, bB=bB: T.matmul(bB.ap, lhsT=wg_ap[:, k, 1024 + j * 128:1024 + (j + 1) * 128], rhs=hTc_ap[:, k, :], start=(k == 0), stop=(k == 7)),
                      reads=[wgb[1]] + hTc, writes=[bB], signal=(k == 7))
            for k in range(4):
                fw.op(pe, lambda k=k, bYA=bYA: T.matmul(bYA.ap, lhsT=wbr_ap[:, k, js], rhs=attT_ap[:, k, c * 512:(c + 1) * 512], start=(k == 0), stop=(k == 3)),
                      reads=[wbrb, attT[k][c]], writes=[bYA], signal=(k == 3))
            for k in range(4):
                fw.op(pe, lambda k=k, bYB=bYB: T.matmul(bYB.ap, lhsT=wbr_ap[:, 4 + k, js], rhs=sguT_ap[:, k, c * 512:(c + 1) * 512], start=(k == 0), stop=(k == 3)),
                      reads=[wbrb] + sguT[4 * c:4 * c + 4], writes=[bYB], signal=(k == 3))
            gA, gB = gAB[(j % 2) * 2], gAB[(j % 2) * 2 + 1]
            fw.op(act, lambda bA=bA, gA=gA: A.activation(out=gA.ap, in_=bA.ap, func=AF.Sigmoid, bias=bgt.ap[:, j:j + 1]), reads=[bA, bgt], writes=[gA])
            fw.op(act, lambda bB=bB, gB=gB: A.activation(out=gB.ap, in_=bB.ap, func=AF.Sigmoid, bias=bgt.ap[:, 8 + j:9 + j]), reads=[bB, bgt], writes=[gB])
            fw.op(dve, lambda gA=gA, bYA=bYA: V.tensor_tensor(out=gA.ap, in0=gA.ap, in1=bYA.ap, op=ALU.mult), reads=[gA, bYA], writes=[gA])
            fw.op(dve, lambda gB=gB, bYB=bYB: V.tensor_tensor(out=gB.ap, in0=gB.ap, in1=bYB.ap, op=ALU.mult), reads=[gB, bYB], writes=[gB])
            fw.op(dve, lambda gA=gA, gB=gB: V.tensor_tensor(out=mixT[j].ap, in0=gA.ap, in1=gB.ap, op=ALU.add), reads=[gA, gB], writes=[mixT[j]])
        for i in range(4):
            t = 4 * c + i
            xo = x1[t % 2]
            for half in range(2):
                bo = banks[half]
                for j in range(8):
                    fw.op(pe, lambda j=j, bo=bo, half=half: T.matmul(bo.ap, lhsT=mixT_ap[:, j, i * 128:(i + 1) * 128], rhs=wo_ap[:, j, half * 512:(half + 1) * 512],
                                                                      start=(j == 0), stop=(j == 7)), reads=[wob] + mixT, writes=[bo], signal=(j == 7))
                fw.op(dve, lambda bo=bo, half=half: V.tensor_tensor(out=xo.ap[:, half * 512:(half + 1) * 512], in0=bo.ap, in1=dxin[i].ap[:, half * 512:(half + 1) * 512], op=ALU.add),
                      reads=[bo, dxin[i]], writes=[xo])
            fw.dma(sp, lambda t=t, xo=xo: SY.dma_start(out=acc_rows[t].ap, in_=xo.ap), reads=[xo], writes=[acc_rows[t]])
            if c == 0 and i == 0:
                pass
            ssb, msb, rsb = Buf(dss_ap[:, 0, NT + t:NT + t + 1]), Buf(dss_ap[:, 1, NT + t:NT + t + 1]), Buf(dss_ap[:, 2, NT + t:NT + t + 1])
            junk = rms_junk
            fw.op(act, lambda xo=xo: A.activation(out=junk.ap, in_=xo.ap, func=AF.Square, accum_out=ssb.ap), reads=[xo], writes=[junk, ssb])
            fw.op(dve, lambda: V.tensor_scalar(out=msb.ap, in0=ssb.ap, scalar1=1.0 / D, scalar2=EPS, op0=ALU.mult, op1=ALU.add), reads=[ssb], writes=[msb])
            fw.op(pool, lambda: G.tensor_tensor(out=rsb.ap, in0=msb.ap, in1=mhalf.ap[:, 0:1], op=ALU.pow), reads=[msb, mhalf], writes=[rsb])
            fw.op(dve, lambda xo=xo: V.scalar_tensor_tensor(out=h2f.ap, in0=xo.ap, scalar=rsb.ap, in1=gffn.ap, op0=ALU.mult, op1=ALU.mult), reads=[xo, rsb, gffn], writes=[h2f])
            hb = h2b[t % 2]
            fw.op(act, lambda hb=hb: A.copy(out=hb.ap, in_=h2f.ap), reads=[h2f], writes=[hb])
            fw.dma(sp, lambda t=t, hb=hb: SY.dma_start(out=h2_d[t * 128:(t + 1) * 128, :], in_=hb.ap), reads=[hb], writes=[h2_all])
            bt = [banks[2], banks[3]]
            for k in range(8):
                fw.op(pe, lambda k=k: T.transpose(out=bt[k // 4].ap[:, (k % 4) * 128:(k % 4 + 1) * 128], in_=h2f.ap[:, k * 128:(k + 1) * 128], identity=identf.ap),
                      reads=[h2f, identf], writes=[bt[k // 4]], signal=(k % 4 == 3))
            fw.op(act, lambda: A.copy(out=h2T.ap[:, 0:4, :], in_=bt[0].ap.rearrange("p (k t) -> p k t", k=4)), reads=[bt[0]], writes=[h2T])
            fw.op(dve, lambda: V.tensor_copy(out=h2T.ap[:, 4:8, :], in_=bt[1].ap.rearrange("p (k t) -> p k t", k=4)), reads=[bt[1], h2T], writes=[h2T])
            bl = banks[6 + (t % 2)]
            for k in range(8):
                fw.op(pe, lambda k=k, bl=bl: T.matmul(bl.ap[:, 0:NE], lhsT=h2T.ap[:, k, :], rhs=wr.ap[:, k, :], start=(k == 0), stop=(k == 7)), reads=[h2T, wr], writes=[bl], signal=(k == 7))
            fw.op(dve, lambda t=t, bl=bl: V.tensor_copy(out=logits_ap[:, :, t], in_=bl.ap[:, 0:NE]), reads=[bl], writes=[logits])

    if dbg:
        o = dout("logits", [128, NE, NT])
        fw.dma(sp, lambda: SY.dma_start(out=o, in_=logits_ap), reads=[logits])
    fw.barrier()
    if dbg:
        o = dout("x1", [S, D])
        tmpb = dxin[0]
        for t in range(NT):
            fw.dma(sp, lambda t=t: SY.dma_start(out=tmpb.ap, in_=acc_d[t * 128:(t + 1) * 128, :]), reads=[acc_rows[t]], writes=[tmpb])
            fw.dma(sp, lambda t=t: SY.dma_start(out=o[t * 128:(t + 1) * 128, :], in_=tmpb.ap), reads=[tmpb])
        fw.barrier()

    if stop_after == "D":
        return finish()
    mb = Bump(nc, base, CK + 96 * KB, ARENA)
    aff2 = Buf(mb.alloc("aff2", [128, NE, NT], F32))
    mx = Buf(mb.alloc("mx", [128, NT], F32))
    sm = Buf(mb.alloc("sm", [128, NT], F32))
    lo = Buf(mb.alloc("lo", [128, NE], F32))
    mid = Buf(mb.alloc("mid", [128, NE], F32))
    cntb = Buf(mb.alloc("cnt", [128, NE], F32))
    cmpb = Buf(mb.alloc("cmp", [128, NE, NT], BF16))
    maskf = Buf(mb.alloc("maskf", [128, NE, NT], F32))
    rank = Buf(mb.alloc("rank", [128, NE, NT], F32))
    cum = Buf(mb.alloc("cum", [128, NE, NT], F32))
    posm = Buf(mb.alloc("posm", [128, NE, NT], F32))
    gtmp = [Buf(mb.alloc(f"gtmp{i}", [128, NE, NT], F32)) for i in range(2)]
    gpb = Buf(mb.alloc("gpb", [128, NE, NT], BF16))
    onesf = Buf(mb.alloc("onesf", [128, NT], F32))
    meta = Buf(mb.alloc("meta", [128, NE, NT, 5], BF16))
    iota = Buf(mb.alloc("iota", [128, 512], F32))
    moe_small_end = mb.pos

    def bc_t(ap2):
        return ap2.unsqueeze(1).to_broadcast([128, NE, NT])

    def bc_e(ap2):
        return ap2.unsqueeze(2).to_broadcast([128, NE, NT])

    pass
    fw.op(dve, lambda: V.tensor_reduce(out=mx.ap, in_=logits_ap.rearrange("p e t -> p t e"), axis=AX.X, op=ALU.max), reads=[logits], writes=[mx])
    fw.op(dve, lambda: V.tensor_tensor(out=aff_ap, in0=logits_ap, in1=bc_t(mx.ap), op=ALU.subtract), reads=[logits, mx], writes=[aff])
    fw.op(act, lambda: A.activation(out=aff_ap, in_=aff_ap, func=AF.Exp), reads=[aff], writes=[aff])
    fw.op(dve, lambda: V.tensor_reduce(out=sm.ap, in_=aff_ap.rearrange("p e t -> p t e"), axis=AX.X, op=ALU.add), reads=[aff], writes=[sm])
    fw.op(dve, lambda: V.reciprocal(out=sm.ap, in_=sm.ap), reads=[sm], writes=[sm])
    fw.op(dve, lambda: V.tensor_tensor(out=aff2.ap, in0=aff_ap, in1=bc_t(sm.ap), op=ALU.mult), reads=[aff, sm], writes=[aff2])
    fw.dma(sp, lambda: SY.dma_start(out=iota.ap, in_=iota_d), writes=[iota])
    fw.dma(sp, lambda: SY.dma_start(out=meta.ap, in_=metac_d), writes=[meta])
    if dbg:
        o = dout("aff", [128, NE, NT])
        fw.dma(sp, lambda: SY.dma_start(out=o, in_=aff2.ap), reads=[aff2])

    fw.op(dve, lambda: V.memset(lo.ap, 0.0), writes=[lo])
    bq_ = banks[7]
    for it in range(NBIS):
        hstep = 2.0 ** (-(it + 1))
        fw.op(dve, lambda: V.tensor_scalar(out=mid.ap, in0=lo.ap, scalar1=hstep, scalar2=None, op0=ALU.add), reads=[lo], writes=[mid])
        fw.op(dve, lambda: V.tensor_tensor(out=cmpb.ap, in0=aff2.ap, in1=bc_e(mid.ap), op=ALU.is_ge), reads=[aff2, mid], writes=[cmpb])
        fw.op(pe, lambda: T.matmul(bq_.ap[:, 0:NE * NT], lhsT=onesb.ap, rhs=cmpb.ap.rearrange("p e t -> p (e t)"), start=True, stop=True), reads=[onesb, cmpb], writes=[bq_])
        fw.op(dve, lambda: V.tensor_reduce(out=cntb.ap, in_=bq_.ap[:, 0:NE * NT].rearrange("p (e t) -> p e t", e=NE), axis=AX.X, op=ALU.add), reads=[bq_], writes=[cntb])
        fw.op(dve, lambda: V.tensor_scalar(out=cntb.ap, in0=cntb.ap, scalar1=CAP - 0.5, scalar2=hstep, op0=ALU.is_ge, op1=ALU.mult), reads=[cntb], writes=[cntb])
        fw.op(dve, lambda: V.tensor_tensor(out=lo.ap, in0=lo.ap, in1=cntb.ap, op=ALU.add), reads=[lo, cntb], writes=[lo])
    fw.op(dve, lambda: V.tensor_tensor(out=cmpb.ap, in0=aff2.ap, in1=bc_e(lo.ap), op=ALU.is_ge), reads=[aff2, lo], writes=[cmpb])
    fw.op(dve, lambda: V.tensor_tensor(out=maskf.ap, in0=aff2.ap, in1=bc_e(lo.ap), op=ALU.is_ge), reads=[aff2, lo], writes=[maskf])
    b6, b7 = banks[6], banks[7]
    flat = lambda ap3: ap3.rearrange("p e t -> p (e t)")
    fw.op(pe, lambda: T.matmul(b6.ap[:, 0:NE * NT], lhsT=utri.ap, rhs=flat(cmpb.ap), start=True, stop=True), reads=[utri, cmpb], writes=[b6])
    fw.op(pe, lambda: T.matmul(b7.ap[:, 0:NE * NT], lhsT=onesb.ap, rhs=flat(cmpb.ap), start=True, stop=True), reads=[onesb, cmpb], writes=[b7])
    fw.op(dve, lambda: V.memset(onesf.ap, 1.0), writes=[onesf])
    fw.op(dve, lambda: V.tensor_copy(out=flat(rank.ap), in_=b7.ap[:, 0:NE * NT]), reads=[b7], writes=[rank])
    for e in range(NE):
        fw.op(dve, lambda e=e: V.tensor_tensor_scan(out=cum.ap[:, e, :], data0=onesf.ap, data1=rank.ap[:, e, :], initial=0.0, op0=ALU.mult, op1=ALU.add),
              reads=[onesf, rank], writes=[cum])
    fw.op(dve, lambda: V.tensor_tensor(out=cum.ap, in0=cum.ap, in1=rank.ap, op=ALU.subtract), reads=[cum, rank], writes=[cum])
    fw.op(dve, lambda: V.tensor_tensor(out=flat(rank.ap), in0=b6.ap[:, 0:NE * NT], in1=flat(cum.ap), op=ALU.add), reads=[b6, cum], writes=[rank])
    fw.op(dve, lambda: V.tensor_tensor(out=posm.ap, in0=rank.ap, in1=maskf.ap, op=ALU.mult), reads=[rank, maskf], writes=[posm])
    fw.op(dve, lambda: V.tensor_scalar(out=posm.ap, in0=posm.ap, scalar1=-1.0, scalar2=None, op0=ALU.add), reads=[posm], writes=[posm])
    g0, g1 = gtmp
    mv_ = meta.ap
    fw.op(dve, lambda: V.tensor_copy(out=gpb.ap, in_=aff2.ap), reads=[aff2], writes=[gpb])
    fw.op(dve, lambda: V.tensor_copy(out=mv_[:, :, :, 2], in_=gpb.ap), reads=[gpb], writes=[meta])
    fw.op(dve, lambda: V.tensor_tensor(out=g0.ap, in0=aff2.ap, in1=gpb.ap, op=ALU.subtract), reads=[aff2, gpb], writes=[g0])
    fw.op(dve, lambda: V.tensor_copy(out=gpb.ap, in_=g0.ap), reads=[g0], writes=[gpb])
    fw.op(dve, lambda: V.tensor_copy(out=mv_[:, :, :, 3], in_=gpb.ap), reads=[gpb], writes=[meta])
    fw.op(dve, lambda: V.tensor_tensor(out=g1.ap, in0=g0.ap, in1=gpb.ap, op=ALU.subtract), reads=[g0, gpb], writes=[g1])
    fw.op(dve, lambda: V.tensor_copy(out=mv_[:, :, :, 4], in_=g1.ap), reads=[g1], writes=[meta])
    if dbg:
        o = dout("posm", [128, NE, NT])
        fw.dma(sp, lambda: SY.dma_start(out=o, in_=posm.ap), reads=[posm])
        o2 = dout("lo", [128, NE])
        fw.dma(sp, lambda: SY.dma_start(out=o2, in_=lo.ap), reads=[lo])

    if stop_after == "R":
        return finish()
    fw.barrier()
    ring_ap = [nc.alloc_sbuf_tensor_at(f"ring{i}", [128, 8192], BF16, offset=base + CK + i * 16 * KB).ap() for i in range(6)]
    ring = [Buf(a) for a in ring_ap]
    mb2 = Bump(nc, base, moe_small_end, ARENA)
    xs = [Buf(mb2.alloc(f"xs{i}", [128, NCT, D], BF16)) for i in range(2)]
    xsT = [Buf(mb2.alloc(f"xsT{i}", [128, 8, CAP], BF16)) for i in range(2)]
    hTe = Buf(mb2.alloc("hTe", [128, 16, CAP], BF16))
    ysb = [Buf(mb2.alloc(f"ysb{i}", [128, D], F32)) for i in range(2)]
    Pt = [Buf(mb2.alloc(f"Pt{i}", [128, CAP], BF16)) for i in range(3)]
    sil = [Buf(mb2.alloc(f"sil{i}", [128, CAP], F32)) for i in range(2)]
    metaT = Buf(mb2.alloc("metaT", [8, CAP], F32))
    metac = [Buf(mb2.alloc(f"metac{i}", [128, NCT, 5], F32)) for i in range(2)]
    idxf = [Buf(mb2.alloc(f"idxf{i}", [128, NCT], F32)) for i in range(2)]
    idxi = [Buf(mb2.alloc(f"idxi{i}", [128, NCT], I32)) for i in range(2)]
    gc = [Buf(mb2.alloc(f"gc{i}", [128, NCT], F32)) for i in range(2)]

    units = []
    for e in range(NE):
        for fb in range(4):
            units.append(("gu", e, fb))
        for half in range(2):
            units.append(("d", e, half))
    unit_issued = [0]

    def issue_unit():
        u = unit_issued[0]
        if u >= len(units):
            return
        unit_issued[0] += 1
        kind, e, i = units[u]
        slot = ring[u % 6]
        sap = ring_ap[u % 6]
        if kind == "gu":
            fw.dma(pool, lambda: G.dma_start(out=sap[:, 0:4096].rearrange("p (k f) -> p k f", k=8),
                                             in_=weg_d[e, :, i * 512:(i + 1) * 512].rearrange("(k p) f -> p k f", p=128)), writes=[slot])
            fw.dma(pool, lambda: G.dma_start(out=sap[:, 4096:8192].rearrange("p (k f) -> p k f", k=8),
                                             in_=weu_d[e, :, i * 512:(i + 1) * 512].rearrange("(k p) f -> p k f", p=128)), reads=[], writes=[])
            slot.w = slot.w + [fw.last_tok]
        else:
            fw.dma(pool, lambda: G.dma_start(out=sap.rearrange("p (k n) -> p k n", k=16),
                                             in_=wed_d[e, :, i * 512:(i + 1) * 512].rearrange("(k p) n -> p k n", p=128)), writes=[slot])

    def unit_index(e, kind, i):
        return e * 6 + (i if kind == "gu" else 4 + i)

    def prep_steps(e):
        s = e % 2
        steps = []
        bm = banks[7]

        def step_t(t):
            p_ = Pt[t % 3]
            fw.op(dve, lambda: V.tensor_scalar(out=p_.ap, in0=iota.ap[:, 0:CAP], scalar1=posm.ap[:, e, t:t + 1], scalar2=None, op0=ALU.is_equal), reads=[iota, posm], writes=[p_])
            fw.op(pe, lambda: T.matmul(bm.ap[0:5, 0:CAP], lhsT=meta.ap[:, e, t, :], rhs=p_.ap, start=(t == 0), stop=(t == NT - 1)), reads=[meta, p_], writes=[bm])

        for t in range(NT):
            steps.append(lambda t=t: step_t(t))

        def fin():
            fw.op(dve, lambda: V.tensor_copy(out=metaT.ap[0:5, :], in_=bm.ap[0:5, 0:CAP]), reads=[bm], writes=[metaT])
            for j in range(NCT):
                fw.op(pe, lambda j=j: T.transpose(out=bm.ap[:, 8 * j:8 * j + 5], in_=metaT.ap[0:5, j * 128:(j + 1) * 128], identity=identf.ap[0:5, 0:5]),
                      reads=[metaT, identf], writes=[bm])
            mc = metac[s]
            fw.op(dve, lambda: V.tensor_copy(out=mc.ap, in_=bm.ap[:, 0:8 * NCT].rearrange("p (j f) -> p j f", f=8)[:, :, 0:5]), reads=[bm], writes=[mc])
            fw.op(dve, lambda: V.scalar_tensor_tensor(out=idxf[s].ap, in0=mc.ap[:, :, 0], scalar=128.0, in1=mc.ap[:, :, 1], op0=ALU.mult, op1=ALU.add), reads=[mc], writes=[idxf[s]])
            fw.op(dve, lambda: V.tensor_copy(out=idxi[s].ap, in_=idxf[s].ap), reads=[idxf[s]], writes=[idxi[s]])
            fw.op(dve, lambda: V.tensor_tensor(out=gc[s].ap, in0=mc.ap[:, :, 2], in1=mc.ap[:, :, 3], op=ALU.add), reads=[mc], writes=[gc[s]])
            fw.op(dve, lambda: V.tensor_tensor(out=gc[s].ap, in0=gc[s].ap, in1=mc.ap[:, :, 4], op=ALU.add), reads=[gc[s], mc], writes=[gc[s]])
            for j in range(NCT):
                fw.dma(pool, lambda j=j: G.indirect_dma_start(out=xs[s].ap[:, j, :], out_offset=None, in_=h2_d,
                                                              in_offset=bass.IndirectOffsetOnAxis(ap=idxi[s].ap[:, j:j + 1], axis=0)),
                       reads=[idxi[s], h2_all], writes=[xs[s]])

        steps.append(fin)
        return steps

    def xs_transposes(e):
        s = e % 2
        bt_ = banks[6]
        pv = bt_.ap.bitcast(BF16)
        for j in range(NCT):
            for k in range(8):
                fw.op(pe, lambda j=j, k=k: T.transpose(out=pv[:, k * 128:(k + 1) * 128], in_=xs[s].ap[:, j, k * 128:(k + 1) * 128], identity=identb.ap),
                      reads=[xs[s], identb], writes=[bt_], signal=(k == 7))
            fw.op(act, lambda j=j: A.copy(out=xsT[s].ap[:, :, j * 128:(j + 1) * 128], in_=pv.rearrange("p (k t) -> p k t", k=8)), reads=[bt_], writes=[xsT[s]])

    pass
    for _ in range(6):
        issue_unit()
    for st in prep_steps(0):
        st()
    xs_transposes(0)
    if dbg:
        o = dout("idx0", [128, NCT], I32)
        fw.dma(sp, lambda: SY.dma_start(out=o, in_=idxi[0].ap), reads=[idxi[0]])
        o2 = dout("gc0", [128, NCT])
        fw.dma(sp, lambda: SY.dma_start(out=o2, in_=gc[0].ap), reads=[gc[0]])

    if stop_after == "P":
        return finish()
    scat_toks = []
    for e in range(NE):
        s = e % 2
        nxt = prep_steps(e + 1) if e + 1 < NE else []
        per = (len(nxt) + 15) // 16 if nxt else 0
        for fc in range(16):
            fb, fl = fc // 4, fc % 4
            u = unit_index(e, "gu", fb)
            slot, sap = ring[u % 6], ring_ap[u % 6]
            wgv = sap[:, 0:4096].rearrange("p (k f) -> p k f", k=8)
            wuv = sap[:, 4096:8192].rearrange("p (k f) -> p k f", k=8)
            ba, bu = banks[fc % 2], banks[2 + fc % 2]
            for k in range(8):
                fw.op(pe, lambda k=k: T.matmul(ba.ap[:, 0:CAP], lhsT=wgv[:, k, fl * 128:(fl + 1) * 128], rhs=xsT[s].ap[:, k, :], start=(k == 0), stop=(k == 7)),
                      reads=[slot, xsT[s]], writes=[ba], signal=(k == 7))
            for k in range(8):
                fw.op(pe, lambda k=k: T.matmul(bu.ap[:, 0:CAP], lhsT=wuv[:, k, fl * 128:(fl + 1) * 128], rhs=xsT[s].ap[:, k, :], start=(k == 0), stop=(k == 7)),
                      reads=[slot, xsT[s]], writes=[bu], signal=(k == 7))
            sl = sil[fc % 2]
            fw.op(act, lambda: A.activation(out=sl.ap, in_=ba.ap[:, 0:CAP], func=AF.Silu), reads=[ba], writes=[sl])
            fw.op(dve, lambda: V.tensor_tensor(out=hTe.ap[:, fc, :], in0=sl.ap, in1=bu.ap[:, 0:CAP], op=ALU.mult), reads=[sl, bu], writes=[hTe])
            if fl == 3:
                issue_unit()
            for _ in range(per):
                if nxt:
                    nxt.pop(0)()
        while nxt:
            nxt.pop(0)()
        if e > 0 and scat_toks:
            pass
        for j in range(NCT):
            yo = ysb[j % 2]
            for half in range(2):
                u = unit_index(e, "d", half)
                slot, sap = ring[u % 6], ring_ap[u % 6]
                wdv = sap.rearrange("p (k n) -> p k n", k=16)
                by = banks[4 + half]
                for fc in range(16):
                    fw.op(pe, lambda fc=fc: T.matmul(by.ap, lhsT=hTe.ap[:, fc, j * 128:(j + 1) * 128], rhs=wdv[:, fc, :], start=(fc == 0), stop=(fc == 15)),
                          reads=[slot, hTe], writes=[by], signal=(fc == 15))
                if half == 0:
                    fw.op(act, lambda: A.activation(out=yo.ap[:, 0:512], in_=by.ap, func=AF.Copy, scale=gc[s].ap[:, j:j + 1]), reads=[by, gc[s]], writes=[yo])
                else:
                    fw.op(dve, lambda: V.tensor_scalar(out=yo.ap[:, 512:1024], in0=by.ap, scalar1=gc[s].ap[:, j:j + 1], scalar2=None, op0=ALU.mult), reads=[by, gc[s], yo], writes=[yo])
            fw.dma(pool, lambda j=j, yo=yo: G.indirect_dma_start(out=acc_d, out_offset=bass.IndirectOffsetOnAxis(ap=idxi[s].ap[:, j:j + 1], axis=0),
                                                                  in_=yo.ap, in_offset=None, compute_op=ALU.add),
                   reads=[yo, idxi[s]] + acc_rows, writes=[])
            scat_toks.append(fw.last_tok)
        for b in acc_rows:
            b.w = list(scat_toks[-NCT:])
            b.r = []
        issue_unit()
        issue_unit()
        if e + 1 < NE:
            xs_transposes(e + 1)

    fw.barrier()
    eb = Bump(nc, base, CK, ARENA)
    fx = [Buf(eb.alloc(f"fx{i}", [128, D], F32)) for i in range(3)]
    fo = [Buf(eb.alloc(f"fo{i}", [128, D], F32)) for i in range(3)]
    fss_ap = eb.alloc("fss", [128, 3, NT], F32)
    rms_junk = Buf(eb.alloc("junke", [128, D], BF16))
    fw.dma(sp, lambda: SY.dma_start(out=gbc.ap, in_=gfin_d.to_broadcast([128, D])), writes=[gbc])
    outb = Buf(out_d)
    for t in range(NT):
        xi, xo = fx[t % 3], fo[t % 3]
        fw.dma(sp, lambda t=t, xi=xi: SY.dma_start(out=xi.ap, in_=acc_d[t * 128:(t + 1) * 128, :]), reads=[acc_rows[t]], writes=[xi])
        ssb, msb, rsb = Buf(fss_ap[:, 0, t:t + 1]), Buf(fss_ap[:, 1, t:t + 1]), Buf(fss_ap[:, 2, t:t + 1])
        rms_tile(xi, ssb, msb, rsb, xo)
        fw.dma(sp, lambda t=t, xo=xo: SY.dma_start(out=out_d[t * 128:(t + 1) * 128, :], in_=xo.ap), reads=[xo], writes=[])
    return finish()


def host_consts(S):
    NT = S // 128
    bf = ml_dtypes.bfloat16
    pos = np.arange(S)
    hi = (pos // 64) * 64
    lo = pos % 64
    qaug = np.zeros((32, S), np.float32)
    qaug[0] = -hi; qaug[1] = -lo; qaug[2] = 1; qaug[3] = 1
    kaugp = np.zeros((32, S), np.float32)
    kaugp[0] = 1; kaugp[1] = 1; kaugp[2] = hi; kaugp[3] = lo
    kaugm = -kaugp
    kk = np.arange(128)[:, None]; qq = np.arange(128)[None, :]
    cdiag = -2.0 * np.maximum(kk - qq, 0)
    utri = (kk <= qq).astype(np.float32)
    iota = np.broadcast_to(np.arange(512, dtype=np.float32), (128, 512)).copy()
    meta = np.zeros((128, NE, NT, 5), np.float32)
    meta[:, :, :, 0] = np.arange(NT)[None, None, :]
    meta[:, :, :, 1] = np.arange(128)[:, None, None]
    return {
        "c_qaug": qaug.astype(bf), "c_kaugp": kaugp.astype(bf), "c_kaugm": kaugm.astype(bf),
        "c_identb": np.eye(128, dtype=np.float32).astype(bf), "c_identf": np.eye(128, dtype=np.float32),
        "c_cdiag": cdiag.astype(np.float32).astype(bf), "c_utri": utri.astype(bf), "c_iota": iota,
        "c_meta": meta.astype(bf),
    }


def make_in_maps(inputs, S):
    f = lambda a: np.ascontiguousarray(np.asarray(a, dtype=np.float32))
    shared = {
        "norm_mix_g": f(inputs["norm_mix_g"]).reshape(1, D),
        "w_in": f(inputs["w_in"])[0],
        "w_gate": f(inputs["w_gate"])[0],
        "bgT": np.ascontiguousarray(f(inputs["b_gate"])[0].reshape(16, 128).T),
        "lam_q1": f(inputs["lam_q1"]).reshape(1, 64), "lam_k1": f(inputs["lam_k1"]).reshape(1, 64),
        "lam_q2": f(inputs["lam_q2"]).reshape(1, 64), "lam_k2": f(inputs["lam_k2"]).reshape(1, 64),
        "subln_gT": np.ascontiguousarray(f(inputs["subln_g"]).reshape(1, 128).T),
        "sgu_ln_g": f(inputs["sgu_ln_g"]).reshape(1, 512), "sgu_ln_b": f(inputs["sgu_ln_b"]).reshape(1, 512),
        "sgu_wT": np.ascontiguousarray(np.transpose(f(inputs["sgu_w"])[0], (2, 0, 1))),
        "sgu_b": f(inputs["sgu_b"]).reshape(1, 512),
        "w_branch": f(inputs["w_branch"])[0], "w_out": f(inputs["w_out"])[0],
        "norm_ffn_g": f(inputs["norm_ffn_g"]).reshape(1, D),
        "w_router": f(inputs["w_router"])[0],
        "w_e_gate": f(inputs["w_e_gate"])[0], "w_e_up": f(inputs["w_e_up"])[0], "w_e_down": f(inputs["w_e_down"])[0],
        "final_norm_g": f(inputs["final_norm_g"]).reshape(1, D),
    }
    shared.update(host_consts(S))
    x = f(inputs["x"])
    return [dict(shared, x=x[b]) for b in range(x.shape[0])]


_CACHE = {}


def kernel(**inputs):
    x = np.asarray(inputs["x"])
    B, S, _ = x.shape
    if S not in _CACHE:
        _CACHE[S] = build(S)[0]
    nc = _CACHE[S]
    in_maps = make_in_maps(inputs, S)
    res = run_bass_kernel_spmd(nc, in_maps, core_ids=list(range(B)))
    return np.stack([np.asarray(r["out"], dtype=np.float32) for r in res.results], axis=0)
```

```python
import math
import numpy as np
import ml_dtypes
import concourse.bass as bass
import concourse.mybir as mybir
from concourse.bass_utils import run_bass_kernel_spmd

F32 = mybir.dt.float32
BF16 = mybir.dt.bfloat16
I32 = mybir.dt.int32
U8 = mybir.dt.uint8
AF = mybir.ActivationFunctionType
ALU = mybir.AluOpType
AX = mybir.AxisListType

D = 1024
H = 4
NE = 16
DFF = 2048
EPS = 1e-6
SLOPES = [2.0 ** (-8.0 * (i + 1) / H) for i in range(H)]
LAMBDA_INIT = 0.8 - 0.6 * math.exp(-0.3 * 0)
ARENA = 207 * 1024
SERIAL = True
NBIS = 27


def _dsz(dt):
    return {F32: 4, BF16: 2, I32: 4, U8: 1}[dt]


class Buf:
    __slots__ = ("ap", "w", "r")

    def __init__(self, ap):
        self.ap = ap
        self.w = []
        self.r = []


class Eng:
    def __init__(self, nc, name, h, nd):
        self.name = name
        self.h = h
        self.is_pe = name == "pe"
        self.sem = nc.alloc_semaphore("sem_" + name)
        self.cnt = 0
        self.seen = {}
        self.dsems = [nc.alloc_semaphore(f"dsem_{name}_{i}") for i in range(nd)]
        self.dcnt = [0] * nd
        self.dnext = 0


class FW:
    def __init__(self, nc):
        self.nc = nc
        self.pe = Eng(nc, "pe", nc.tensor, 0)
        self.act = Eng(nc, "act", nc.scalar, 0)
        self.dve = Eng(nc, "dve", nc.vector, 0)
        self.pool = Eng(nc, "pool", nc.gpsimd, 40)
        self.sp = Eng(nc, "sp", nc.sync, 40)
        self.engs = [self.pe, self.act, self.dve, self.pool, self.sp]
        self.by_name = {e.name: e for e in self.engs}
        self.last_tok = None
        self.glast = None

    def _wait(self, eng, tok):
        sem, val, key = tok
        if eng.seen.get(key, 0) >= val:
            return
        if key in self.by_name:
            assert val <= self.by_name[key].cnt, ("wait on not-yet-emitted signal", key, val, self.by_name[key].cnt)
        eng.h.wait_ge(sem, val)
        eng.seen[key] = val

    def _deps(self, eng, reads, writes):
        for b in reads:
            for t in b.w:
                if eng.is_pe and t[2] == "pe":
                    continue
                self._wait(eng, t)
        for b in writes:
            for t in b.w + b.r:
                if t[2] == eng.name:
                    continue
                self._wait(eng, t)

    def _commit(self, tok, reads, writes):
        for b in reads:
            b.r = [t for t in b.r if t[2] != tok[2]] + [tok]
        for b in writes:
            b.w = [tok]
            b.r = []

    def op(self, eng, fn, reads=(), writes=(), signal=True):
        self._deps(eng, reads, writes)
        if SERIAL and not eng.is_pe:
            if self.glast is not None and self.glast[2] != eng.name:
                self._wait(eng, self.glast)
        inst = fn()
        if signal:
            eng.cnt += 1
            inst.then_inc(eng.sem, 1)
            tok = (eng.sem, eng.cnt, eng.name)
        else:
            tok = (eng.sem, eng.cnt + 1, eng.name)
        self._commit(tok, reads, writes)
        if not eng.is_pe:
            self.glast = tok
        return tok

    def dma(self, eng, fn, reads=(), writes=()):
        self._deps(eng, reads, writes)
        i = eng.dnext
        eng.dnext = (eng.dnext + 1) % len(eng.dsems)
        sem = eng.dsems[i]
        key = f"d_{eng.name}_{i}"
        if eng.dcnt[i] > 0:
            self._wait(eng, (sem, eng.dcnt[i], key))
        inst = fn()
        eng.dcnt[i] += 16
        inst.then_inc(sem, 16)
        tok = (sem, eng.dcnt[i], key)
        self._commit(tok, reads, writes)
        self.last_tok = tok
        return tok

    def barrier(self):
        toks = []
        for e in self.engs:
            if e.cnt:
                toks.append((e.sem, e.cnt, e.name))
            for i, s in enumerate(e.dsems):
                if e.dcnt[i]:
                    toks.append((s, e.dcnt[i], f"d_{e.name}_{i}"))
        for e in self.engs:
            for t in toks:
                if t[2] == e.name:
                    continue
                self._wait(e, t)


class Bump:
    def __init__(self, nc, base, start, end):
        self.nc = nc
        self.base = base
        self.pos = start
        self.end = end
        self.n = 0

    def alloc(self, name, shape, dt):
        nbytes = int(np.prod(shape[1:])) * _dsz(dt)
        nbytes = (nbytes + 31) // 32 * 32
        off = self.pos
        self.pos += nbytes
        assert self.pos <= self.end, (name, self.pos, self.end)
        Bump_counter[0] += 1
        return self.nc.alloc_sbuf_tensor_at(f"{name}_{Bump_counter[0]}", list(shape), dt, offset=self.base + off).ap()


Bump_counter = [0]


def build(S, dbg=False, stop_after=None):
    NT = S // 128
    NCH = S // 512
    CAP = 2 * S // NE
    NCT = CAP // 128
    assert S % 512 == 0 and CAP % 128 == 0 and CAP <= 512

    nc = bass.Bass("TRN2", target_bir_lowering=False)
    fw = FW(nc)
    pe, act, dve, pool, sp = fw.pe, fw.act, fw.dve, fw.pool, fw.sp
    V = nc.vector
    A = nc.scalar
    T = nc.tensor
    G = nc.gpsimd
    SY = nc.sync

    def finish():
        for e_ in fw.engs:
            if e_.cnt and e_ is not sp:
                sp.h.wait_ge(e_.sem, e_.cnt)
        for e_ in (sp, pool):
            for i, sm_ in enumerate(e_.dsems):
                if e_.dcnt[i]:
                    sp.h.wait_ge(sm_, e_.dcnt[i])
        return nc, dbg_out

    def din(name, shape, dt=F32):
        return nc.dram_tensor(name, list(shape), dt, kind="ExternalInput").ap()

    x_d = din("x", [S, D])
    gmix_d = din("norm_mix_g", [1, D])
    w_in_d = din("w_in", [D, 2560])
    w_gate_d = din("w_gate", [D, 2 * D])
    bgT_d = din("bgT", [128, 16])
    lam_d = [din(n, [1, 64]) for n in ("lam_q1", "lam_k1", "lam_q2", "lam_k2")]
    subg_d = din("subln_gT", [128, 1])
    lng_d = din("sgu_ln_g", [1, 512])
    lnb_d = din("sgu_ln_b", [1, 512])
    wsT_d = din("sgu_wT", [128, 4, 128])
    bs_d = din("sgu_b", [1, 512])
    wbr_d = din("w_branch", [D, D])
    wout_d = din("w_out", [D, D])
    gffn_d = din("norm_ffn_g", [1, D])
    wr_d = din("w_router", [D, NE])
    weg_d = din("w_e_gate", [NE, D, DFF])
    weu_d = din("w_e_up", [NE, D, DFF])
    wed_d = din("w_e_down", [NE, DFF, D])
    gfin_d = din("final_norm_g", [1, D])
    qaug_d = din("c_qaug", [32, S], BF16)
    kaugp_d = din("c_kaugp", [32, S], BF16)
    kaugm_d = din("c_kaugm", [32, S], BF16)
    identb_d = din("c_identb", [128, 128], BF16)
    identf_d = din("c_identf", [128, 128])
    cdiag_d = din("c_cdiag", [128, 128], BF16)
    utri_d = din("c_utri", [128, 128], BF16)
    iota_d = din("c_iota", [128, 512])
    metac_d = din("c_meta", [128, NE, NT, 5], BF16)

    out_d = nc.dram_tensor("out", [S, D], F32, kind="ExternalOutput").ap()
    acc_d = nc.dram_tensor("acc_d", [S, D], F32).ap()
    h2_d = nc.dram_tensor("h2_d", [S, D], BF16).ap()
    dbg_out = {}

    def dout(name, shape, dt=F32):
        ap = nc.dram_tensor("dbg_" + name, list(shape), dt, kind="ExternalOutput").ap()
        dbg_out[name] = ap
        return ap

    nc.alloc_sbuf_tensor("arena", [128, ARENA], U8)
    base = nc.lookup_mloc("arena").addr
    KB = 1024

    banks = [Buf(nc.alloc_psum_tensor(f"bank{i}", [128, 512], F32).ap()) for i in range(8)]

    CK = 16 * KB
    cst = Bump(nc, base, 0, CK)
    identb = Buf(cst.alloc("identb", [128, 128], BF16))
    identf = Buf(cst.alloc("identf", [128, 128], F32))
    onesb = Buf(cst.alloc("onesb", [128, 128], BF16))
    cdiag = Buf(cst.alloc("cdiag", [128, 128], BF16))
    utri = Buf(cst.alloc("utri", [128, 128], BF16))
    mhalf = Buf(cst.alloc("mhalf", [128, 8], F32))
    gbc = Buf(cst.alloc("gbc", [128, D], F32))
    neglam = Buf(cst.alloc("neglam", [128, 1], F32))
    gsub = Buf(cst.alloc("gsub", [128, 1], F32))
    small = Buf(cst.alloc("small", [128, 8], F32))
    epsb = Buf(cst.alloc("epsb", [128, 1], F32))
    lamt = Buf(cst.alloc("lamt", [128, 4, 64], F32))
    logits_ap = cst.alloc("logits", [128, NE, NT], F32)
    logits = Buf(logits_ap)
    aff_ap = cst.alloc("aff", [128, NE, NT], F32)
    aff = Buf(aff_ap)

    fw.dma(sp, lambda: SY.dma_start(out=identb.ap, in_=identb_d), writes=[identb])
    fw.dma(sp, lambda: SY.dma_start(out=identf.ap, in_=identf_d), writes=[identf])
    fw.dma(sp, lambda: SY.dma_start(out=cdiag.ap, in_=cdiag_d), writes=[cdiag])
    fw.dma(sp, lambda: SY.dma_start(out=utri.ap, in_=utri_d), writes=[utri])
    fw.dma(sp, lambda: SY.dma_start(out=gbc.ap, in_=gmix_d.to_broadcast([128, D])), writes=[gbc])
    for i in range(4):
        fw.dma(sp, lambda i=i: SY.dma_start(out=lamt.ap[:, i, :], in_=lam_d[i].to_broadcast([128, 64])), writes=[lamt])
    fw.dma(sp, lambda: SY.dma_start(out=gsub.ap, in_=subg_d), writes=[gsub])
    fw.op(dve, lambda: V.memset(onesb.ap, 1.0), writes=[onesb])
    fw.op(dve, lambda: V.memset(epsb.ap, EPS), writes=[epsb])
    pass
    fw.op(dve, lambda: V.memset(mhalf.ap, -0.5), writes=[mhalf])
    for i_ in range(2):
        fw.op(dve, lambda i_=i_: V.tensor_tensor(out=lamt.ap[:, 2 * i_, :], in0=lamt.ap[:, 2 * i_, :], in1=lamt.ap[:, 2 * i_ + 1, :], op=ALU.mult), reads=[lamt], writes=[lamt])
        fw.op(dve, lambda i_=i_: V.tensor_reduce(out=small.ap[:, i_:i_ + 1], in_=lamt.ap[:, 2 * i_, :], axis=AX.X, op=ALU.add), reads=[lamt], writes=[small])
    fw.op(act, lambda: A.activation(out=small.ap[:, 2:4], in_=small.ap[:, 0:2], func=AF.Exp), reads=[small], writes=[small])
    fw.op(dve, lambda: V.tensor_tensor(out=small.ap[:, 4:5], in0=small.ap[:, 3:4], in1=small.ap[:, 2:3], op=ALU.subtract), reads=[small], writes=[small])
    fw.op(dve, lambda: V.tensor_scalar(out=neglam.ap, in0=small.ap[:, 4:5], scalar1=-LAMBDA_INIT, scalar2=None, op0=ALU.add), reads=[small], writes=[neglam])
    fw.op(dve, lambda: V.tensor_scalar(out=gsub.ap, in0=gsub.ap, scalar1=1.0 - LAMBDA_INIT, scalar2=None, op0=ALU.mult), reads=[gsub], writes=[gsub])

    def rms_tile(xin, ssb, msb, rsb, outb, out_dtype_bf16_buf=None):
        junk = rms_junk
        fw.op(act, lambda: A.activation(out=junk.ap, in_=xin.ap, func=AF.Square, accum_out=ssb.ap), reads=[xin], writes=[junk, ssb])
        fw.op(dve, lambda: V.tensor_scalar(out=msb.ap, in0=ssb.ap, scalar1=1.0 / D, scalar2=EPS, op0=ALU.mult, op1=ALU.add), reads=[ssb], writes=[msb])
        fw.op(pool, lambda: G.tensor_tensor(out=rsb.ap, in0=msb.ap, in1=mhalf.ap[:, 0:1], op=ALU.pow), reads=[msb, mhalf], writes=[rsb])
        fw.op(dve, lambda: V.scalar_tensor_tensor(out=outb.ap, in0=xin.ap, scalar=rsb.ap, in1=gbc.ap, op0=ALU.mult, op1=ALU.mult),
              reads=[xin, rsb, gbc], writes=[outb])

    def transpose_bf_tile(src, bank, dst_ap, dstbuf, evac_eng):
        pv = bank.ap.bitcast(BF16)
        for k in range(8):
            fw.op(pe, lambda k=k: T.transpose(out=pv[:, k * 128:(k + 1) * 128], in_=src.ap[:, k * 128:(k + 1) * 128], identity=identb.ap),
                  reads=[src, identb], writes=[bank], signal=(k == 7))
        src_v = pv.rearrange("p (k t) -> p k t", k=8)
        if evac_eng is act:
            fw.op(act, lambda: A.copy(out=dst_ap, in_=src_v), reads=[bank], writes=[dstbuf])
        else:
            fw.op(dve, lambda: V.tensor_copy(out=dst_ap, in_=src_v), reads=[bank], writes=[dstbuf])

    attT_ap = nc.alloc_sbuf_tensor_at("attT", [128, 4, S], BF16, offset=base + CK).ap()
    hT_off = CK + S * 8
    hT_ap = nc.alloc_sbuf_tensor_at("hT", [128, 8, S], BF16, offset=base + hT_off).ap()
    attT = [[Buf(attT_ap[:, h, c * 512:(c + 1) * 512]) for c in range(NCH)] for h in range(H)]
    hT = [Buf(hT_ap[:, :, t * 128:(t + 1) * 128]) for t in range(NT)]

    def hT_chunk_bufs(c):
        return hT[4 * c:4 * c + 4]

    ab = Bump(nc, base, hT_off + S * 16, ARENA)
    rms_junk = Buf(ab.alloc("junk", [128, D], BF16))
    qk_ap = {}
    qk = {}
    for nm in ("Q0", "Q1", "K0p", "K0m", "K1p", "K1m"):
        ap_ = ab.alloc(nm, [96, S], BF16)
        qk_ap[nm] = ap_
        qk[nm] = [Buf(ap_[0:64, c * 512:(c + 1) * 512]) for c in range(NCH)]
    aug = {nm: Buf(qk_ap[nm][64:96, :]) for nm in qk_ap}
    Vh_ap = ab.alloc("Vh", [128, NT, 128], BF16)
    Vh = [Buf(Vh_ap[:, 4 * c:4 * c + 4, :]) for c in range(NCH)]
    ptile = [[Buf(ab.alloc(f"pt{i}{j}", [128, 512], BF16)) for j in range(2)] for i in range(2)]
    wqkv = [Buf(ab.alloc(f"wqkv{i}", [128, 8, 384], BF16)) for i in range(1)] * 2
    xin = [Buf(ab.alloc(f"xin{i}", [128, D], F32)) for i in range(2)]
    xn = [Buf(ab.alloc(f"xn{i}", [128, D], BF16)) for i in range(2)]
    ss_ap = ab.alloc("ss", [128, NT], F32)
    ms_ap = ab.alloc("ms", [128, NT], F32)
    rs_ap = ab.alloc("rs", [128, NT], F32)
    cmb = [Buf(ab.alloc(f"cmb{i}", [128, 512], F32)) for i in range(5)]
    sqb = Buf(ab.alloc("sqb", [128, 512], BF16))

    for nm, src in (("Q0", qaug_d), ("Q1", qaug_d), ("K0p", kaugp_d), ("K1p", kaugp_d), ("K0m", kaugm_d), ("K1m", kaugm_d)):
        fw.dma(sp, lambda nm=nm, src=src: SY.dma_start(out=qk_ap[nm][64:96, :], in_=src), writes=[aug[nm]])

    def load_wqkv(h):
        wb = wqkv[h % 2]
        for j, off in enumerate((0, 512, 1024)):
            fw.dma(pool, lambda j=j, off=off: G.dma_start(out=wb.ap[:, :, j * 128:(j + 1) * 128],
                                                          in_=w_in_d[:, off + h * 128: off + (h + 1) * 128].rearrange("(k p) n -> p k n", p=128)),
                   writes=[wb])

    if stop_after == "A0":
        return finish()
    load_wqkv(0)
    if stop_after == "A1":
        return finish()
    for t in range(NT):
        if stop_after == "A2" and t == 1:
            return finish()
        xi = xin[t % 2]
        fw.dma(sp, lambda t=t, xi=xi: SY.dma_start(out=xi.ap, in_=x_d[t * 128:(t + 1) * 128, :]), writes=[xi])
        ssb, msb, rsb = Buf(ss_ap[:, t:t + 1]), Buf(ms_ap[:, t:t + 1]), Buf(rs_ap[:, t:t + 1])
        rms_tile(xi, ssb, msb, rsb, xn[t % 2])
        transpose_bf_tile(xn[t % 2], banks[t % 2], hT[t].ap, hT[t], act)

    if dbg:
        o = dout("hT", [128, 8, S], BF16)
        fw.dma(sp, lambda: SY.dma_start(out=o, in_=hT_ap), reads=hT)

    if stop_after == "A":
        return finish()
    sb_banks = [[banks[0], banks[1]], [banks[2], banks[3]]]
    accO = [banks[4], banks[6]]
    accL = [banks[5], banks[7]]
    for h in range(H):
        wb = wqkv[h % 2]
        qscale = 1.0 / (8.0 * SLOPES[h])
        pbank = 0
        for c in range(NCH):
            hcb = hT_chunk_bufs(c)
            bq = banks[pbank % 4]; pbank += 1
            for k in range(8):
                fw.op(pe, lambda k=k, bq=bq: T.matmul(bq.ap, lhsT=wb.ap[:, k, 0:128], rhs=hT_ap[:, k, c * 512:(c + 1) * 512], start=(k == 0), stop=(k == 7)),
                      reads=[wb] + hcb, writes=[bq], signal=(k == 7))
            fw.op(act, lambda bq=bq: A.activation(out=qk["Q0"][c].ap, in_=bq.ap[0:64, :], func=AF.Copy, scale=qscale), reads=[bq], writes=[qk["Q0"][c]])
            fw.op(dve, lambda bq=bq: V.tensor_scalar(out=qk["Q1"][c].ap, in0=bq.ap[64:128, :], scalar1=qscale, scalar2=None, op0=ALU.mult), reads=[bq], writes=[qk["Q1"][c]])
            bk = banks[pbank % 4]; pbank += 1
            for k in range(8):
                fw.op(pe, lambda k=k, bk=bk: T.matmul(bk.ap, lhsT=wb.ap[:, k, 128:256], rhs=hT_ap[:, k, c * 512:(c + 1) * 512], start=(k == 0), stop=(k == 7)),
                      reads=[wb] + hcb, writes=[bk], signal=(k == 7))
            fw.op(act, lambda bk=bk: A.copy(out=qk["K0p"][c].ap, in_=bk.ap[0:64, :]), reads=[bk], writes=[qk["K0p"][c]])
            fw.op(dve, lambda bk=bk: V.tensor_copy(out=qk["K0m"][c].ap, in_=bk.ap[0:64, :]), reads=[bk], writes=[qk["K0m"][c]])
            fw.op(act, lambda bk=bk: A.copy(out=qk["K1p"][c].ap, in_=bk.ap[64:128, :]), reads=[bk], writes=[qk["K1p"][c]])
            fw.op(dve, lambda bk=bk: V.tensor_copy(out=qk["K1m"][c].ap, in_=bk.ap[64:128, :]), reads=[bk], writes=[qk["K1m"][c]])
            bv = banks[pbank % 4]; pbank += 1
            for i in range(4):
                t = 4 * c + i
                for k in range(8):
                    fw.op(pe, lambda k=k, i=i, t=t, bv=bv: T.matmul(bv.ap[:, i * 128:(i + 1) * 128], lhsT=hT_ap[:, k, t * 128:(t + 1) * 128], rhs=wb.ap[:, k, 256:384],
                                                                     start=(k == 0), stop=(k == 7)), reads=[wb, hT[t]], writes=[bv], signal=(k == 7 and i == 3))
            fw.op(dve, lambda bv=bv: V.tensor_copy(out=Vh[c].ap, in_=bv.ap.rearrange("p (a b) -> p a b", a=4)), reads=[bv], writes=[Vh[c]])

        if stop_after == "B0":
            return finish()
        if h + 1 < H:
            load_wqkv(h + 1)
        slope = SLOPES[h]

        def scores(qc, kt, par):
            kc = kt // 4
            ks = slice(kt * 128, (kt + 1) * 128)
            for comp in range(2):
                bnk = sb_banks[par][comp]
                Qn = "Q%d" % comp
                if kc != qc:
                    Kn = ("K%dp" if qc > kc else "K%dm") % comp
                    fw.op(pe, lambda bnk=bnk, Kn=Kn, Qn=Qn: T.matmul(bnk.ap, lhsT=qk_ap[Kn][0:96, ks], rhs=qk_ap[Qn][0:96, qc * 512:(qc + 1) * 512], start=True, stop=True),
                          reads=[qk[Kn][kc], qk[Qn][qc], aug[Kn], aug[Qn]], writes=[bnk])
                else:
                    kb = kt % 4
                    order = [i for i in range(4) if i != kb] + [kb]
                    for i in order:
                        Kn = ("K%dp" if i >= kb else "K%dm") % comp
                        qs = slice(qc * 512 + i * 128, qc * 512 + (i + 1) * 128)
                        fw.op(pe, lambda bnk=bnk, Kn=Kn, Qn=Qn, qs=qs, i=i: T.matmul(bnk.ap[:, i * 128:(i + 1) * 128], lhsT=qk_ap[Kn][0:96, ks], rhs=qk_ap[Qn][0:96, qs],
                                                                                      start=True, stop=(i != kb)),
                              reads=[qk[Kn][kc], qk[Qn][qc], aug[Kn], aug[Qn]], writes=[bnk], signal=False)
                        if i == kb:
                            fw.op(pe, lambda bnk=bnk, i=i: T.matmul(bnk.ap[:, i * 128:(i + 1) * 128], lhsT=identb.ap, rhs=cdiag.ap, start=False, stop=True),
                                  reads=[identb, cdiag], writes=[bnk])

        for qc in range(NCH):
            scores(qc, 0, 0)
            for kt in range(NT):
                par = kt % 2
                if kt + 1 < NT:
                    scores(qc, kt + 1, 1 - par)
                for comp in range(2):
                    bnk = sb_banks[par][comp]
                    pt = ptile[par][comp]
                    fw.op(act, lambda bnk=bnk, pt=pt: A.activation(out=pt.ap, in_=bnk.ap, func=AF.Exp, scale=slope), reads=[bnk], writes=[pt])
                for comp in range(2):
                    pt = ptile[par][comp]
                    fw.op(pe, lambda pt=pt, comp=comp: T.matmul(accO[comp].ap, lhsT=Vh_ap[:, kt, :], rhs=pt.ap, start=(kt == 0), stop=(kt == NT - 1)),
                          reads=[pt, Vh[kt // 4]], writes=[accO[comp]], signal=(kt == NT - 1))
                    fw.op(pe, lambda pt=pt, comp=comp: T.matmul(accL[comp].ap, lhsT=onesb.ap, rhs=pt.ap, start=(kt == 0), stop=(kt == NT - 1)),
                          reads=[pt, onesb], writes=[accL[comp]], signal=(kt == NT - 1))
            if stop_after == "B1":
                return finish()
            r1, t1, r2, t2, aa = cmb
            fw.op(dve, lambda: V.reciprocal(out=r1.ap, in_=accL[0].ap), reads=[accL[0]], writes=[r1])
            fw.op(dve, lambda: V.tensor_tensor(out=t1.ap, in0=accO[0].ap, in1=r1.ap, op=ALU.mult), reads=[accO[0], r1], writes=[t1])
            fw.op(dve, lambda: V.reciprocal(out=r2.ap, in_=accL[1].ap), reads=[accL[1]], writes=[r2])
            fw.op(dve, lambda: V.tensor_tensor(out=t2.ap, in0=accO[1].ap, in1=r2.ap, op=ALU.mult), reads=[accO[1], r2], writes=[t2])
            fw.op(dve, lambda: V.scalar_tensor_tensor(out=aa.ap, in0=t2.ap, scalar=neglam.ap, in1=t1.ap, op0=ALU.mult, op1=ALU.add), reads=[t2, t1, neglam], writes=[aa])
            fw.op(dve, lambda: V.tensor_tensor(out=sqb.ap, in0=aa.ap, in1=aa.ap, op=ALU.mult), reads=[aa], writes=[sqb])
            sbk = sb_banks[0][0]
            fw.op(pe, lambda: T.matmul(sbk.ap, lhsT=onesb.ap, rhs=sqb.ap, start=True, stop=True), reads=[onesb, sqb], writes=[sbk])
            fw.op(act, lambda: A.activation(out=r1.ap, in_=sbk.ap, func=AF.Ln, scale=1.0 / 128, bias=epsb.ap), reads=[sbk, epsb], writes=[r1])
            fw.op(act, lambda: A.activation(out=r2.ap, in_=r1.ap, func=AF.Exp, scale=-0.5), reads=[r1], writes=[r2])
            fw.op(dve, lambda: V.scalar_tensor_tensor(out=attT[h][qc].ap, in0=aa.ap, scalar=gsub.ap, in1=r2.ap, op0=ALU.mult, op1=ALU.mult),
                  reads=[aa, gsub, r2], writes=[attT[h][qc]])
            if stop_after == "B2":
                return finish()

    if dbg:
        o = dout("attT", [128, 4, S], BF16)
        fw.dma(sp, lambda: SY.dma_start(out=o, in_=attT_ap), reads=[b for hb in attT for b in hb])

    if stop_after == "B":
        return finish()
    fw.barrier()
    sgu_off = hT_off + S * 16
    sguT_ap = nc.alloc_sbuf_tensor_at("sguT", [128, 4, S], BF16, offset=base + sgu_off).ap()
    sguT = [Buf(sguT_ap[:, :, t * 128:(t + 1) * 128]) for t in range(NT)]
    cb = Bump(nc, base, sgu_off + S * 8, ARENA)
    wu = Buf(cb.alloc("wu", [128, 8, 512], BF16))
    wsw = Buf(cb.alloc("ws", [128, 8, 512], BF16))
    wsT = Buf(cb.alloc("wsT", [128, 4, 128], BF16))
    lng = Buf(cb.alloc("lng", [128, 512], F32))
    lnb = Buf(cb.alloc("lnb", [128, 512], F32))
    bsb = Buf(cb.alloc("bsb", [128, 512], F32))
    uT = [Buf(cb.alloc(f"uT{i}", [128, 4, 512], BF16)) for i in range(2)]
    gs = [Buf(cb.alloc(f"gs{i}", [128, 512], F32)) for i in range(2)]
    sc1 = [Buf(cb.alloc(f"sc1{i}", [128, 512], F32)) for i in range(2)]
    scb = [Buf(cb.alloc(f"scb{i}", [128, 512], BF16)) for i in range(2)]
    zb = [Buf(cb.alloc(f"zb{i}", [128, 512], F32)) for i in range(2)]
    st_ap = cb.alloc("bnst", [128, NT, 6], F32)
    mv_ap = cb.alloc("bnmv", [128, NT, 2], F32)
    lms_ap = cb.alloc("lms", [128, NT], F32)
    lrs_ap = cb.alloc("lrs", [128, NT], F32)

    fw.dma(pool, lambda: G.dma_start(out=wu.ap, in_=w_in_d[:, 1536:2048].rearrange("(k p) n -> p k n", p=128)), writes=[wu])
    fw.dma(pool, lambda: G.dma_start(out=wsw.ap, in_=w_in_d[:, 2048:2560].rearrange("(k p) n -> p k n", p=128)), writes=[wsw])
    fw.dma(pool, lambda: G.dma_start(out=wsT.ap, in_=wsT_d), writes=[wsT])
    fw.dma(sp, lambda: SY.dma_start(out=lng.ap, in_=lng_d.to_broadcast([128, 512])), writes=[lng])
    fw.dma(sp, lambda: SY.dma_start(out=lnb.ap, in_=lnb_d.to_broadcast([128, 512])), writes=[lnb])
    fw.dma(sp, lambda: SY.dma_start(out=bsb.ap, in_=bs_d.to_broadcast([128, 512])), writes=[bsb])

    pass
    for c in range(NCH):
        hcb = hT_chunk_bufs(c)
        u = uT[c % 2]
        for cc in range(4):
            bu = banks[cc]
            for k in range(8):
                fw.op(pe, lambda k=k, cc=cc, bu=bu: T.matmul(bu.ap, lhsT=wu.ap[:, k, cc * 128:(cc + 1) * 128], rhs=hT_ap[:, k, c * 512:(c + 1) * 512], start=(k == 0), stop=(k == 7)),
                      reads=[wu] + hcb, writes=[bu], signal=(k == 7))
            fw.op(act, lambda cc=cc, bu=bu: A.activation(out=u.ap[:, cc, :], in_=bu.ap, func=AF.Gelu), reads=[bu], writes=[u])
        for i in range(4):
            t = 4 * c + i
            bs_ = banks[4 + (t % 2)]
            bz = banks[6 + (t % 2)]
            g_, s1_, sb_, z_ = gs[t % 2], sc1[t % 2], scb[t % 2], zb[t % 2]
            for k in range(8):
                fw.op(pe, lambda k=k, t=t, bs_=bs_: T.matmul(bs_.ap, lhsT=hT_ap[:, k, t * 128:(t + 1) * 128], rhs=wsw.ap[:, k, :], start=(k == 0), stop=(k == 7)),
                      reads=[wsw, hT[t]], writes=[bs_], signal=(k == 7))
            fw.op(act, lambda: A.activation(out=g_.ap, in_=bs_.ap, func=AF.Gelu), reads=[bs_], writes=[g_])
            stb, mvb = Buf(st_ap[:, t, :]), Buf(mv_ap[:, t, :])
            lmsb, lrsb = Buf(lms_ap[:, t:t + 1]), Buf(lrs_ap[:, t:t + 1])
            fw.op(dve, lambda: V.bn_stats(out=stb.ap, in_=g_.ap), reads=[g_], writes=[stb])
            fw.op(dve, lambda: V.bn_aggr(out=mvb.ap, in_=stb.ap), reads=[stb], writes=[mvb])
            fw.op(dve, lambda: V.tensor_scalar(out=lmsb.ap, in0=mvb.ap[:, 1:2], scalar1=EPS, scalar2=None, op0=ALU.add), reads=[mvb], writes=[lmsb])
            fw.op(pool, lambda: G.tensor_tensor(out=lrsb.ap, in0=lmsb.ap, in1=mhalf.ap[:, 0:1], op=ALU.pow), reads=[lmsb, mhalf], writes=[lrsb])
            fw.op(dve, lambda: V.tensor_scalar(out=s1_.ap, in0=g_.ap, scalar1=mvb.ap[:, 0:1], scalar2=lrsb.ap, op0=ALU.subtract, op1=ALU.mult),
                  reads=[g_, mvb, lrsb], writes=[s1_])
            fw.op(dve, lambda: V.tensor_tensor(out=s1_.ap, in0=s1_.ap, in1=lng.ap, op=ALU.mult), reads=[s1_, lng], writes=[s1_])
            fw.op(dve, lambda: V.tensor_tensor(out=sb_.ap, in0=s1_.ap, in1=lnb.ap, op=ALU.add), reads=[s1_, lnb], writes=[sb_])
            for g in range(4):
                fw.op(pe, lambda g=g: T.matmul(bz.ap[:, g * 128:(g + 1) * 128], lhsT=sb_.ap[:, g * 128:(g + 1) * 128], rhs=wsT.ap[:, g, :], start=True, stop=True),
                      reads=[sb_, wsT], writes=[bz], signal=(g == 3))
            fw.op(dve, lambda: V.tensor_tensor(out=z_.ap, in0=bz.ap, in1=bsb.ap, op=ALU.add), reads=[bz, bsb], writes=[z_])
            fw.op(dve, lambda i=i: V.tensor_tensor(out=sguT[t].ap, in0=z_.ap.rearrange("p (g t) -> p g t", g=4), in1=u.ap[:, :, i * 128:(i + 1) * 128], op=ALU.mult),
                  reads=[z_, u], writes=[sguT[t]])

    if dbg:
        o = dout("sguT", [128, 4, S], BF16)
        fw.dma(sp, lambda: SY.dma_start(out=o, in_=sguT_ap), reads=sguT)

    if stop_after == "C":
        return finish()
    fw.barrier()
    if S * 16 >= 64 * KB:
        w_off, d_start = hT_off, sgu_off + S * 8
    else:
        w_off = sgu_off + S * 8
        d_start = w_off + 64 * KB
    wg_ap = nc.alloc_sbuf_tensor_at("wg", [128, 8, 2048], BF16, offset=base + w_off).ap()
    wbr_ap = nc.alloc_sbuf_tensor_at("wbr", [128, 8, 1024], BF16, offset=base + w_off + 32 * KB).ap()
    wo_ap = nc.alloc_sbuf_tensor_at("wo", [128, 8, 1024], BF16, offset=base + w_off + 48 * KB).ap()
    db = Bump(nc, base, d_start, ARENA)
    wgb = [Buf(wg_ap[:, :, i * 1024:(i + 1) * 1024]) for i in range(2)]
    wbrb = Buf(wbr_ap)
    wob = Buf(wo_ap)
    for i in range(2):
        fw.dma(pool, lambda i=i: G.dma_start(out=wgb[i].ap, in_=w_gate_d[:, i * 1024:(i + 1) * 1024].rearrange("(k p) n -> p k n", p=128)), writes=[wgb[i]])
    fw.dma(pool, lambda: G.dma_start(out=wbrb.ap, in_=wbr_d.rearrange("(k p) n -> p k n", p=128)), writes=[wbrb])
    fw.dma(pool, lambda: G.dma_start(out=wob.ap, in_=wout_d.rearrange("(k p) n -> p k n", p=128)), writes=[wob])
    bgt = Buf(db.alloc("bgt", [128, 16], F32))
    wr = Buf(db.alloc("wr", [128, 8, NE], F32))
    gffn = Buf(db.alloc("gffn", [128, D], F32))
    dxin = [Buf(db.alloc(f"dxin{i}", [128, D], F32)) for i in range(4)]
    dxn = [Buf(db.alloc(f"dxn{i}", [128, D], BF16)) for i in range(1)] * 2
    hTc_ap = db.alloc("hTc", [128, 8, 512], BF16)
    hTc = [Buf(hTc_ap[:, :, i * 128:(i + 1) * 128]) for i in range(4)]
    mixT_ap = db.alloc("mixT", [128, 8, 512], BF16)
    mixT = [Buf(mixT_ap[:, j, :]) for j in range(8)]
    gAB = [Buf(db.alloc(f"gAB{i}", [128, 512], F32)) for i in range(2)] * 2
    x1 = [Buf(db.alloc(f"x1{i}", [128, D], F32)) for i in range(1)] * 2
    h2f = Buf(db.alloc("h2f", [128, D], F32))
    h2b = [Buf(db.alloc(f"h2b{i}", [128, D], BF16)) for i in range(1)] * 2
    h2T = Buf(db.alloc("h2T", [128, 8, 128], F32))
    dss_ap = db.alloc("dss", [128, 3, 2 * NT], F32)
    rms_junk = Buf(db.alloc("junkd", [128, D], BF16))
    fw.dma(sp, lambda: SY.dma_start(out=bgt.ap, in_=bgT_d), writes=[bgt])
    fw.dma(sp, lambda: SY.dma_start(out=wr.ap, in_=wr_d.rearrange("(k p) e -> p k e", p=128)), writes=[wr])
    fw.dma(sp, lambda: SY.dma_start(out=gffn.ap, in_=gffn_d.to_broadcast([128, D])), writes=[gffn])
    acc_rows = [Buf(acc_d[t * 128:(t + 1) * 128, :]) for t in range(NT)]
    h2_all = Buf(h2_d)

    pass
    for c in range(NCH):
        for i in range(4):
            t = 4 * c + i
            fw.dma(sp, lambda t=t, i=i: SY.dma_start(out=dxin[i].ap, in_=x_d[t * 128:(t + 1) * 128, :]), writes=[dxin[i]])
            ssb, msb, rsb = Buf(dss_ap[:, 0, t:t + 1]), Buf(dss_ap[:, 1, t:t + 1]), Buf(dss_ap[:, 2, t:t + 1])
            rms_tile(dxin[i], ssb, msb, rsb, dxn[t % 2])
            transpose_bf_tile(dxn[t % 2], banks[t % 2], hTc[i].ap, hTc[i], act)
        for j in range(8):
            bA = banks[(j % 2) * 4 + 0]
            bB = banks[(j % 2) * 4 + 1]
            bYA = banks[(j % 2) * 4 + 2]
            bYB = banks[(j % 2) * 4 + 3]
            js = slice(j * 128, (j + 1) * 128)
            for k in range(8):
                fw.op(pe, lambda k=k, bA=bA: T.matmul(bA.ap, lhsT=wg_ap[:, k, j * 128:(j + 1) * 128], rhs=hTc_ap[:, k, :], start=(k == 0), stop=(k == 7)),
                      reads=[wgb[0]] + hTc, writes=[bA], signal=(k == 7))
            for k in range(8):
                fw.op(pe, lambda k=k, bB=bB: T.matmul(bB.ap, lhsT=wg_ap[:, k, 1024 + j * 128:1024 + (j + 1) * 128], rhs=hTc_ap[:, k, :], start=(k == 0), stop=(k == 7)),
                      reads=[wgb[1]] + hTc, writes=[bB], signal=(k == 7))
            for k in range(4):
                fw.op(pe, lambda k=k, bYA=bYA: T.matmul(bYA.ap, lhsT=wbr_ap[:, k, js], rhs=attT_ap[:, k, c * 512:(c + 1) * 512], start=(k == 0), stop=(k == 3)),
                      reads=[wbrb, attT[k][c]], writes=[bYA], signal=(k == 3))
            for k in range(4):
                fw.op(pe, lambda k=k, bYB=bYB: T.matmul(bYB.ap, lhsT=wbr_ap[:, 4 + k, js], rhs=sguT_ap[:, k, c * 512:(c + 1) * 512], start=(k == 0), stop=(k == 3)),
                      reads=[wbrb] + sguT[4 * c:4 * c + 4], writes=[bYB], signal=(k == 3))
            gA, gB = gAB[(j % 2) * 2], gAB[(j % 2) * 2 + 1]
            fw.op(act, lambda bA=bA, gA=gA: A.activation(out=gA.ap, in_=bA.ap, func=AF.Sigmoid, bias=bgt.ap[:, j:j + 1]), reads=[bA, bgt], writes=[gA])
            fw.op(act, lambda bB=bB, gB=gB: A.activation(out=gB.ap, in_=bB.ap, func=AF.Sigmoid, bias=bgt.ap[:, 8 + j:9 + j]), reads=[bB, bgt], writes=[gB])
            fw.op(dve, lambda gA=gA, bYA=bYA: V.tensor_tensor(out=gA.ap, in0=gA.ap, in1=bYA.ap, op=ALU.mult), reads=[gA, bYA], writes=[gA])
            fw.op(dve, lambda gB=gB, bYB=bYB: V.tensor_tensor(out=gB.ap, in0=gB.ap, in1=bYB.ap, op=ALU.mult), reads=[gB, bYB], writes=[gB])
            fw.op(dve, lambda gA=gA, gB=gB: V.tensor_tensor(out=mixT[j].ap, in0=gA.ap, in1=gB.ap, op=ALU.add), reads=[gA, gB], writes=[mixT[j]])
        for i in range(4):
            t = 4 * c + i
            xo = x1[t % 2]
            for half in range(2):
                bo = banks[half]
                for j in range(8):
                    fw.op(pe, lambda j=j, bo=bo, half=half: T.matmul(bo.ap, lhsT=mixT_ap[:, j, i * 128:(i + 1) * 128], rhs=wo_ap[:, j, half * 512:(half + 1) * 512],
                                                                      start=(j == 0), stop=(j == 7)), reads=[wob] + mixT, writes=[bo], signal=(j == 7))
                fw.op(dve, lambda bo=bo, half=half: V.tensor_tensor(out=xo.ap[:, half * 512:(half + 1) * 512], in0=bo.ap, in1=dxin[i].ap[:, half * 512:(half + 1) * 512], op=ALU.add),
                      reads=[bo, dxin[i]], writes=[xo])
            fw.dma(sp, lambda t=t, xo=xo: SY.dma_start(out=acc_rows[t].ap, in_=xo.ap), reads=[xo], writes=[acc_rows[t]])
            if c == 0 and i == 0:
                pass
            ssb, msb, rsb = Buf(dss_ap[:, 0, NT + t:NT + t + 1]), Buf(dss_ap[:, 1, NT + t:NT + t + 1]), Buf(dss_ap[:, 2, NT + t:NT + t + 1])
            junk = rms_junk
            fw.op(act, lambda xo=xo: A.activation(out=junk.ap, in_=xo.ap, func=AF.Square, accum_out=ssb.ap), reads=[xo], writes=[junk, ssb])
            fw.op(dve, lambda: V.tensor_scalar(out=msb.ap, in0=ssb.ap, scalar1=1.0 / D, scalar2=EPS, op0=ALU.mult, op1=ALU.add), reads=[ssb], writes=[msb])
            fw.op(pool, lambda: G.tensor_tensor(out=rsb.ap, in0=msb.ap, in1=mhalf.ap[:, 0:1], op=ALU.pow), reads=[msb, mhalf], writes=[rsb])
            fw.op(dve, lambda xo=xo: V.scalar_tensor_tensor(out=h2f.ap, in0=xo.ap, scalar=rsb.ap, in1=gffn.ap, op0=ALU.mult, op1=ALU.mult), reads=[xo, rsb, gffn], writes=[h2f])
            hb = h2b[t % 2]
            fw.op(act, lambda hb=hb: A.copy(out=hb.ap, in_=h2f.ap), reads=[h2f], writes=[hb])
            fw.dma(sp, lambda t=t, hb=hb: SY.dma_start(out=h2_d[t * 128:(t + 1) * 128, :], in_=hb.ap), reads=[hb], writes=[h2_all])
            bt = [banks[2], banks[3]]
            for k in range(8):
                fw.op(pe, lambda k=k: T.transpose(out=bt[k // 4].ap[:, (k % 4) * 128:(k % 4 + 1) * 128], in_=h2f.ap[:, k * 128:(k + 1) * 128], identity=identf.ap),
                      reads=[h2f, identf], writes=[bt[k // 4]], signal=(k % 4 == 3))
            fw.op(act, lambda: A.copy(out=h2T.ap[:, 0:4, :], in_=bt[0].ap.rearrange("p (k t) -> p k t", k=4)), reads=[bt[0]], writes=[h2T])
            fw.op(dve, lambda: V.tensor_copy(out=h2T.ap[:, 4:8, :], in_=bt[1].ap.rearrange("p (k t) -> p k t", k=4)), reads=[bt[1], h2T], writes=[h2T])
            bl = banks[6 + (t % 2)]
            for k in range(8):
                fw.op(pe, lambda k=k, bl=bl: T.matmul(bl.ap[:, 0:NE], lhsT=h2T.ap[:, k, :], rhs=wr.ap[:, k, :], start=(k == 0), stop=(k == 7)), reads=[h2T, wr], writes=[bl], signal=(k == 7))
            fw.op(dve, lambda t=t, bl=bl: V.tensor_copy(out=logits_ap[:, :, t], in_=bl.ap[:, 0:NE]), reads=[bl], writes=[logits])

    if dbg:
        o = dout("logits", [128, NE, NT])
        fw.dma(sp, lambda: SY.dma_start(out=o, in_=logits_ap), reads=[logits])
    fw.barrier()
    if dbg:
        o = dout("x1", [S, D])
        tmpb = dxin[0]
        for t in range(NT):
            fw.dma(sp, lambda t=t: SY.dma_start(out=tmpb.ap, in_=acc_d[t * 128:(t + 1) * 128, :]), reads=[acc_rows[t]], writes=[tmpb])
            fw.dma(sp, lambda t=t: SY.dma_start(out=o[t * 128:(t + 1) * 128, :], in_=tmpb.ap), reads=[tmpb])
        fw.barrier()

    if stop_after == "D":
        return finish()
    ring_ap = [nc.alloc_sbuf_tensor_at(f"ring{i}", [128, 8192], BF16, offset=base + CK + i * 16 * KB).ap() for i in range(6)]
    ring = [Buf(a) for a in ring_ap]
    units = []
    for e in range(NE):
        for fb in range(4):
            units.append(("gu", e, fb))
        for half in range(2):
            units.append(("d", e, half))
    unit_issued = [0]

    def issue_unit():
        u = unit_issued[0]
        if u >= len(units):
            return
        unit_issued[0] += 1
        kind, e, i = units[u]
        slot = ring[u % 6]
        sap = ring_ap[u % 6]
        if kind == "gu":
            fw.dma(pool, lambda: G.dma_start(out=sap[:, 0:4096].rearrange("p (k f) -> p k f", k=8),
                                             in_=weg_d[e, :, i * 512:(i + 1) * 512].rearrange("(k p) f -> p k f", p=128)), writes=[slot])
            fw.dma(pool, lambda: G.dma_start(out=sap[:, 4096:8192].rearrange("p (k f) -> p k f", k=8),
                                             in_=weu_d[e, :, i * 512:(i + 1) * 512].rearrange("(k p) f -> p k f", p=128)), reads=[], writes=[])
            slot.w = slot.w + [fw.last_tok]
        else:
            fw.dma(pool, lambda: G.dma_start(out=sap.rearrange("p (k n) -> p k n", k=16),
                                             in_=wed_d[e, :, i * 512:(i + 1) * 512].rearrange("(k p) n -> p k n", p=128)), writes=[slot])

    for _ in range(6):
        issue_unit()
    mb = Bump(nc, base, CK + 96 * KB, ARENA)
    aff2 = Buf(mb.alloc("aff2", [128, NE, NT], F32))
    mx = Buf(mb.alloc("mx", [128, NT], F32))
    sm = Buf(mb.alloc("sm", [128, NT], F32))
    lo = Buf(mb.alloc("lo", [128, NE], F32))
    mid = Buf(mb.alloc("mid", [128, NE], F32))
    cntb = Buf(mb.alloc("cnt", [128, NE], F32))
    cmpb = Buf(mb.alloc("cmp", [128, NE, NT], BF16))
    maskf = Buf(mb.alloc("maskf", [128, NE, NT], F32))
    rank = Buf(mb.alloc("rank", [128, NE, NT], F32))
    cum = Buf(mb.alloc("cum", [128, NE, NT], F32))
    posm = Buf(mb.alloc("posm", [128, NE, NT], F32))
    gtmp = [Buf(mb.alloc(f"gtmp{i}", [128, NE, NT], F32)) for i in range(2)]
    gpb = Buf(mb.alloc("gpb", [128, NE, NT], BF16))
    onesf = Buf(mb.alloc("onesf", [128, NT], F32))
    meta = Buf(mb.alloc("meta", [128, NE, NT, 5], BF16))
    iota = Buf(mb.alloc("iota", [128, 512], F32))
    moe_small_end = mb.pos

    def bc_t(ap2):
        return ap2.unsqueeze(1).to_broadcast([128, NE, NT])

    def bc_e(ap2):
        return ap2.unsqueeze(2).to_broadcast([128, NE, NT])

    pass
    fw.op(dve, lambda: V.tensor_reduce(out=mx.ap, in_=logits_ap.rearrange("p e t -> p t e"), axis=AX.X, op=ALU.max), reads=[logits], writes=[mx])
    fw.op(dve, lambda: V.tensor_tensor(out=aff_ap, in0=logits_ap, in1=bc_t(mx.ap), op=ALU.subtract), reads=[logits, mx], writes=[aff])
    fw.op(act, lambda: A.activation(out=aff_ap, in_=aff_ap, func=AF.Exp), reads=[aff], writes=[aff])
    fw.op(dve, lambda: V.tensor_reduce(out=sm.ap, in_=aff_ap.rearrange("p e t -> p t e"), axis=AX.X, op=ALU.add), reads=[aff], writes=[sm])
    fw.op(dve, lambda: V.reciprocal(out=sm.ap, in_=sm.ap), reads=[sm], writes=[sm])
    fw.op(dve, lambda: V.tensor_tensor(out=aff2.ap, in0=aff_ap, in1=bc_t(sm.ap), op=ALU.mult), reads=[aff, sm], writes=[aff2])
    fw.dma(sp, lambda: SY.dma_start(out=iota.ap, in_=iota_d), writes=[iota])
    fw.dma(sp, lambda: SY.dma_start(out=meta.ap, in_=metac_d), writes=[meta])
    if dbg:
        o = dout("aff", [128, NE, NT])
        fw.dma(sp, lambda: SY.dma_start(out=o, in_=aff2.ap), reads=[aff2])

    fw.op(dve, lambda: V.memset(lo.ap, 0.0), writes=[lo])
    bq_ = banks[7]
    for it in range(NBIS):
        hstep = 2.0 ** (-(it + 1))
        fw.op(dve, lambda: V.tensor_scalar(out=mid.ap, in0=lo.ap, scalar1=hstep, scalar2=None, op0=ALU.add), reads=[lo], writes=[mid])
        fw.op(dve, lambda: V.tensor_tensor(out=cmpb.ap, in0=aff2.ap, in1=bc_e(mid.ap), op=ALU.is_ge), reads=[aff2, mid], writes=[cmpb])
        fw.op(pe, lambda: T.matmul(bq_.ap[:, 0:NE * NT], lhsT=onesb.ap, rhs=cmpb.ap.rearrange("p e t -> p (e t)"), start=True, stop=True), reads=[onesb, cmpb], writes=[bq_])
        fw.op(dve, lambda: V.tensor_reduce(out=cntb.ap, in_=bq_.ap[:, 0:NE * NT].rearrange("p (e t) -> p e t", e=NE), axis=AX.X, op=ALU.add), reads=[bq_], writes=[cntb])
        fw.op(dve, lambda: V.tensor_scalar(out=cntb.ap, in0=cntb.ap, scalar1=CAP - 0.5, scalar2=hstep, op0=ALU.is_ge, op1=ALU.mult), reads=[cntb], writes=[cntb])
        fw.op(dve, lambda: V.tensor_tensor(out=lo.ap, in0=lo.ap, in1=cntb.ap, op=ALU.add), reads=[lo, cntb], writes=[lo])
    fw.op(dve, lambda: V.tensor_tensor(out=cmpb.ap, in0=aff2.ap, in1=bc_e(lo.ap), op=ALU.is_ge), reads=[aff2, lo], writes=[cmpb])
    fw.op(dve, lambda: V.tensor_tensor(out=maskf.ap, in0=aff2.ap, in1=bc_e(lo.ap), op=ALU.is_ge), reads=[aff2, lo], writes=[maskf])
    b6, b7 = banks[6], banks[7]
    flat = lambda ap3: ap3.rearrange("p e t -> p (e t)")
    fw.op(pe, lambda: T.matmul(b6.ap[:, 0:NE * NT], lhsT=utri.ap, rhs=flat(cmpb.ap), start=True, stop=True), reads=[utri, cmpb], writes=[b6])
    fw.op(pe, lambda: T.matmul(b7.ap[:, 0:NE * NT], lhsT=onesb.ap, rhs=flat(cmpb.ap), start=True, stop=True), reads=[onesb, cmpb], writes=[b7])
    fw.op(dve, lambda: V.memset(onesf.ap, 1.0), writes=[onesf])
    fw.op(dve, lambda: V.tensor_copy(out=flat(rank.ap), in_=b7.ap[:, 0:NE * NT]), reads=[b7], writes=[rank])
    for e in range(NE):
        fw.op(dve, lambda e=e: V.tensor_tensor_scan(out=cum.ap[:, e, :], data0=onesf.ap, data1=rank.ap[:, e, :], initial=0.0, op0=ALU.mult, op1=ALU.add),
              reads=[onesf, rank], writes=[cum])
    fw.op(dve, lambda: V.tensor_tensor(out=cum.ap, in0=cum.ap, in1=rank.ap, op=ALU.subtract), reads=[cum, rank], writes=[cum])
    fw.op(dve, lambda: V.tensor_tensor(out=flat(rank.ap), in0=b6.ap[:, 0:NE * NT], in1=flat(cum.ap), op=ALU.add), reads=[b6, cum], writes=[rank])
    fw.op(dve, lambda: V.tensor_tensor(out=posm.ap, in0=rank.ap, in1=maskf.ap, op=ALU.mult), reads=[rank, maskf], writes=[posm])
    fw.op(dve, lambda: V.tensor_scalar(out=posm.ap, in0=posm.ap, scalar1=-1.0, scalar2=None, op0=ALU.add), reads=[posm], writes=[posm])
    g0, g1 = gtmp
    mv_ = meta.ap
    fw.op(dve, lambda: V.tensor_copy(out=gpb.ap, in_=aff2.ap), reads=[aff2], writes=[gpb])
    fw.op(dve, lambda: V.tensor_copy(out=mv_[:, :, :, 2], in_=gpb.ap), reads=[gpb], writes=[meta])
    fw.op(dve, lambda: V.tensor_tensor(out=g0.ap, in0=aff2.ap, in1=gpb.ap, op=ALU.subtract), reads=[aff2, gpb], writes=[g0])
    fw.op(dve, lambda: V.tensor_copy(out=gpb.ap, in_=g0.ap), reads=[g0], writes=[gpb])
    fw.op(dve, lambda: V.tensor_copy(out=mv_[:, :, :, 3], in_=gpb.ap), reads=[gpb], writes=[meta])
    fw.op(dve, lambda: V.tensor_tensor(out=g1.ap, in0=g0.ap, in1=gpb.ap, op=ALU.subtract), reads=[g0, gpb], writes=[g1])
    fw.op(dve, lambda: V.tensor_copy(out=mv_[:, :, :, 4], in_=g1.ap), reads=[g1], writes=[meta])
    if dbg:
        o = dout("posm", [128, NE, NT])
        fw.dma(sp, lambda: SY.dma_start(out=o, in_=posm.ap), reads=[posm])
        o2 = dout("lo", [128, NE])
        fw.dma(sp, lambda: SY.dma_start(out=o2, in_=lo.ap), reads=[lo])

    if stop_after == "R":
        return finish()
    fw.barrier()
    mb2 = Bump(nc, base, moe_small_end, ARENA)
    xs = [Buf(mb2.alloc(f"xs{i}", [128, NCT, D], BF16)) for i in range(2)]
    xsT = [Buf(mb2.alloc(f"xsT{i}", [128, 8, CAP], BF16)) for i in range(2)]
    hTe = Buf(mb2.alloc("hTe", [128, 16, CAP], BF16))
    ysb = [Buf(mb2.alloc(f"ysb{i}", [128, D], F32)) for i in range(2)]
    Pt = [Buf(mb2.alloc(f"Pt{i}", [128, CAP], BF16)) for i in range(3)]
    sil = [Buf(mb2.alloc(f"sil{i}", [128, CAP], F32)) for i in range(2)]
    metaT = Buf(mb2.alloc("metaT", [8, CAP], F32))
    metac = [Buf(mb2.alloc(f"metac{i}", [128, NCT, 5], F32)) for i in range(2)]
    idxf = [Buf(mb2.alloc(f"idxf{i}", [128, NCT], F32)) for i in range(2)]
    idxi = [Buf(mb2.alloc(f"idxi{i}", [128, NCT], I32)) for i in range(2)]
    gc = [Buf(mb2.alloc(f"gc{i}", [128, NCT], F32)) for i in range(2)]

    def unit_index(e, kind, i):
        return e * 6 + (i if kind == "gu" else 4 + i)

    def prep_steps(e):
        s = e % 2
        steps = []
        bm = banks[7]

        def step_t(t):
            p_ = Pt[t % 3]
            fw.op(dve, lambda: V.tensor_scalar(out=p_.ap, in0=iota.ap[:, 0:CAP], scalar1=posm.ap[:, e, t:t + 1], scalar2=None, op0=ALU.is_equal), reads=[iota, posm], writes=[p_])
            fw.op(pe, lambda: T.matmul(bm.ap[0:5, 0:CAP], lhsT=meta.ap[:, e, t, :], rhs=p_.ap, start=(t == 0), stop=(t == NT - 1)), reads=[meta, p_], writes=[bm])

        for t in range(NT):
            steps.append(lambda t=t: step_t(t))

        def fin():
            fw.op(dve, lambda: V.tensor_copy(out=metaT.ap[0:5, :], in_=bm.ap[0:5, 0:CAP]), reads=[bm], writes=[metaT])
            for j in range(NCT):
                fw.op(pe, lambda j=j: T.transpose(out=bm.ap[:, 8 * j:8 * j + 5], in_=metaT.ap[0:5, j * 128:(j + 1) * 128], identity=identf.ap[0:5, 0:5]),
                      reads=[metaT, identf], writes=[bm])
            mc = metac[s]
            fw.op(dve, lambda: V.tensor_copy(out=mc.ap, in_=bm.ap[:, 0:8 * NCT].rearrange("p (j f) -> p j f", f=8)[:, :, 0:5]), reads=[bm], writes=[mc])
            fw.op(dve, lambda: V.scalar_tensor_tensor(out=idxf[s].ap, in0=mc.ap[:, :, 0], scalar=128.0, in1=mc.ap[:, :, 1], op0=ALU.mult, op1=ALU.add), reads=[mc], writes=[idxf[s]])
            fw.op(dve, lambda: V.tensor_copy(out=idxi[s].ap, in_=idxf[s].ap), reads=[idxf[s]], writes=[idxi[s]])
            fw.op(dve, lambda: V.tensor_tensor(out=gc[s].ap, in0=mc.ap[:, :, 2], in1=mc.ap[:, :, 3], op=ALU.add), reads=[mc], writes=[gc[s]])
            fw.op(dve, lambda: V.tensor_tensor(out=gc[s].ap, in0=gc[s].ap, in1=mc.ap[:, :, 4], op=ALU.add), reads=[gc[s], mc], writes=[gc[s]])
            for j in range(NCT):
                fw.dma(pool, lambda j=j: G.indirect_dma_start(out=xs[s].ap[:, j, :], out_offset=None, in_=h2_d,
                                                              in_offset=bass.IndirectOffsetOnAxis(ap=idxi[s].ap[:, j:j + 1], axis=0)),
                       reads=[idxi[s], h2_all], writes=[xs[s]])

        steps.append(fin)
        return steps

    def xs_transposes(e):
        s = e % 2
        bt_ = banks[6]
        pv = bt_.ap.bitcast(BF16)
        for j in range(NCT):
            for k in range(8):
                fw.op(pe, lambda j=j, k=k: T.transpose(out=pv[:, k * 128:(k + 1) * 128], in_=xs[s].ap[:, j, k * 128:(k + 1) * 128], identity=identb.ap),
                      reads=[xs[s], identb], writes=[bt_], signal=(k == 7))
            fw.op(act, lambda j=j: A.copy(out=xsT[s].ap[:, :, j * 128:(j + 1) * 128], in_=pv.rearrange("p (k t) -> p k t", k=8)), reads=[bt_], writes=[xsT[s]])

    pass
    for st in prep_steps(0):
        st()
    xs_transposes(0)
    if dbg:
        o = dout("idx0", [128, NCT], I32)
        fw.dma(sp, lambda: SY.dma_start(out=o, in_=idxi[0].ap), reads=[idxi[0]])
        o2 = dout("gc0", [128, NCT])
        fw.dma(sp, lambda: SY.dma_start(out=o2, in_=gc[0].ap), reads=[gc[0]])

    if stop_after == "P":
        return finish()
    scat_toks = []
    for e in range(NE):
        s = e % 2
        nxt = prep_steps(e + 1) if e + 1 < NE else []
        per = (len(nxt) + 15) // 16 if nxt else 0
        for fc in range(16):
            fb, fl = fc // 4, fc % 4
            u = unit_index(e, "gu", fb)
            slot, sap = ring[u % 6], ring_ap[u % 6]
            wgv = sap[:, 0:4096].rearrange("p (k f) -> p k f", k=8)
            wuv = sap[:, 4096:8192].rearrange("p (k f) -> p k f", k=8)
            ba, bu = banks[fc % 2], banks[2 + fc % 2]
            for k in range(8):
                fw.op(pe, lambda k=k: T.matmul(ba.ap[:, 0:CAP], lhsT=wgv[:, k, fl * 128:(fl + 1) * 128], rhs=xsT[s].ap[:, k, :], start=(k == 0), stop=(k == 7)),
                      reads=[slot, xsT[s]], writes=[ba], signal=(k == 7))
            for k in range(8):
                fw.op(pe, lambda k=k: T.matmul(bu.ap[:, 0:CAP], lhsT=wuv[:, k, fl * 128:(fl + 1) * 128], rhs=xsT[s].ap[:, k, :], start=(k == 0), stop=(k == 7)),
                      reads=[slot, xsT[s]], writes=[bu], signal=(k == 7))
            sl = sil[fc % 2]
            fw.op(act, lambda: A.activation(out=sl.ap, in_=ba.ap[:, 0:CAP], func=AF.Silu), reads=[ba], writes=[sl])
            fw.op(dve, lambda: V.tensor_tensor(out=hTe.ap[:, fc, :], in0=sl.ap, in1=bu.ap[:, 0:CAP], op=ALU.mult), reads=[sl, bu], writes=[hTe])
            if fl == 3:
                issue_unit()
            for _ in range(per):
                if nxt:
                    nxt.pop(0)()
        while nxt:
            nxt.pop(0)()
        if e > 0 and scat_toks:
            pass
        for j in range(NCT):
            yo = ysb[j % 2]
            for half in range(2):
                u = unit_index(e, "d", half)
                slot, sap = ring[u % 6], ring_ap[u % 6]
                wdv = sap.rearrange("p (k n) -> p k n", k=16)
                by = banks[4 + half]
                for fc in range(16):
                    fw.op(pe, lambda fc=fc: T.matmul(by.ap, lhsT=hTe.ap[:, fc, j * 128:(j + 1) * 128], rhs=wdv[:, fc, :], start=(fc == 0), stop=(fc == 15)),
                          reads=[slot, hTe], writes=[by], signal=(fc == 15))
                if half == 0:
                    fw.op(act, lambda: A.activation(out=yo.ap[:, 0:512], in_=by.ap, func=AF.Copy, scale=gc[s].ap[:, j:j + 1]), reads=[by, gc[s]], writes=[yo])
                else:
                    fw.op(dve, lambda: V.tensor_scalar(out=yo.ap[:, 512:1024], in0=by.ap, scalar1=gc[s].ap[:, j:j + 1], scalar2=None, op0=ALU.mult), reads=[by, gc[s], yo], writes=[yo])
            fw.dma(pool, lambda j=j, yo=yo: G.indirect_dma_start(out=acc_d, out_offset=bass.IndirectOffsetOnAxis(ap=idxi[s].ap[:, j:j + 1], axis=0),
                                                                  in_=yo.ap, in_offset=None, compute_op=ALU.add),
                   reads=[yo, idxi[s]] + acc_rows, writes=[])
            scat_toks.append(fw.last_tok)
        for b in acc_rows:
            b.w = list(scat_toks[-NCT:])
            b.r = []
        issue_unit()
        issue_unit()
        if e + 1 < NE:
            xs_transposes(e + 1)

    fw.barrier()
    eb = Bump(nc, base, CK, ARENA)
    fx = [Buf(eb.alloc(f"fx{i}", [128, D], F32)) for i in range(3)]
    fo = [Buf(eb.alloc(f"fo{i}", [128, D], F32)) for i in range(3)]
    fss_ap = eb.alloc("fss", [128, 3, NT], F32)
    rms_junk = Buf(eb.alloc("junke", [128, D], BF16))
    fw.dma(sp, lambda: SY.dma_start(out=gbc.ap, in_=gfin_d.to_broadcast([128, D])), writes=[gbc])
    outb = Buf(out_d)
    for t in range(NT):
        xi, xo = fx[t % 3], fo[t % 3]
        fw.dma(sp, lambda t=t, xi=xi: SY.dma_start(out=xi.ap, in_=acc_d[t * 128:(t + 1) * 128, :]), reads=[acc_rows[t]], writes=[xi])
        ssb, msb, rsb = Buf(fss_ap[:, 0, t:t + 1]), Buf(fss_ap[:, 1, t:t + 1]), Buf(fss_ap[:, 2, t:t + 1])
        rms_tile(xi, ssb, msb, rsb, xo)
        fw.dma(sp, lambda t=t, xo=xo: SY.dma_start(out=out_d[t * 128:(t + 1) * 128, :], in_=xo.ap), reads=[xo], writes=[])
    return finish()


def host_consts(S):
    NT = S // 128
    bf = ml_dtypes.bfloat16
    pos = np.arange(S)
    hi = (pos // 64) * 64
    lo = pos % 64
    qaug = np.zeros((32, S), np.float32)
    qaug[0] = -hi; qaug[1] = -lo; qaug[2] = 1; qaug[3] = 1
    kaugp = np.zeros((32, S), np.float32)
    kaugp[0] = 1; kaugp[1] = 1; kaugp[2] = hi; kaugp[3] = lo
    kaugm = -kaugp
    kk = np.arange(128)[:, None]; qq = np.arange(128)[None, :]
    cdiag = -2.0 * np.maximum(kk - qq, 0)
    utri = (kk <= qq).astype(np.float32)
    iota = np.broadcast_to(np.arange(512, dtype=np.float32), (128, 512)).copy()
    meta = np.zeros((128, NE, NT, 5), np.float32)
    meta[:, :, :, 0] = np.arange(NT)[None, None, :]
    meta[:, :, :, 1] = np.arange(128)[:, None, None]
    return {
        "c_qaug": qaug.astype(bf), "c_kaugp": kaugp.astype(bf), "c_kaugm": kaugm.astype(bf),
        "c_identb": np.eye(128, dtype=np.float32).astype(bf), "c_identf": np.eye(128, dtype=np.float32),
        "c_cdiag": cdiag.astype(np.float32).astype(bf), "c_utri": utri.astype(bf), "c_iota": iota,
        "c_meta": meta.astype(bf),
    }


def make_in_maps(inputs, S):
    f = lambda a: np.ascontiguousarray(np.asarray(a, dtype=np.float32))
    shared = {
        "norm_mix_g": f(inputs["norm_mix_g"]).reshape(1, D),
        "w_in": f(inputs["w_in"])[0],
        "w_gate": f(inputs["w_gate"])[0],
        "bgT": np.ascontiguousarray(f(inputs["b_gate"])[0].reshape(16, 128).T),
        "lam_q1": f(inputs["lam_q1"]).reshape(1, 64), "lam_k1": f(inputs["lam_k1"]).reshape(1, 64),
        "lam_q2": f(inputs["lam_q2"]).reshape(1, 64), "lam_k2": f(inputs["lam_k2"]).reshape(1, 64),
        "subln_gT": np.ascontiguousarray(f(inputs["subln_g"]).reshape(1, 128).T),
        "sgu_ln_g": f(inputs["sgu_ln_g"]).reshape(1, 512), "sgu_ln_b": f(inputs["sgu_ln_b"]).reshape(1, 512),
        "sgu_wT": np.ascontiguousarray(np.transpose(f(inputs["sgu_w"])[0], (2, 0, 1))),
        "sgu_b": f(inputs["sgu_b"]).reshape(1, 512),
        "w_branch": f(inputs["w_branch"])[0], "w_out": f(inputs["w_out"])[0],
        "norm_ffn_g": f(inputs["norm_ffn_g"]).reshape(1, D),
        "w_router": f(inputs["w_router"])[0],
        "w_e_gate": f(inputs["w_e_gate"])[0], "w_e_up": f(inputs["w_e_up"])[0], "w_e_down": f(inputs["w_e_down"])[0],
        "final_norm_g": f(inputs["final_norm_g"]).reshape(1, D),
    }
    shared.update(host_consts(S))
    x = f(inputs["x"])
    return [dict(shared, x=x[b]) for b in range(x.shape[0])]


_CACHE = {}


def kernel(**inputs):
    x = np.asarray(inputs["x"])
    B, S, _ = x.shape
    if S not in _CACHE:
        _CACHE[S] = build(S)[0]
    nc = _CACHE[S]
    in_maps = make_in_maps(inputs, S)
    res = run_bass_kernel_spmd(nc, in_maps, core_ids=list(range(B)))
    return np.stack([np.asarray(r["out"], dtype=np.float32) for r in res.results], axis=0)
```
